# Optimizing a Trainium2 kernel written in Bass

```python
import jax, jax.numpy as jnp
from jax import lax
import numpy as np

D_MODEL = 2048
BATCH = 8
SEQ = 2048
DEPTH = 2

HEAD_DIM = 128
A_WIDTH = D_MODEL // 2
A_HEADS = A_WIDTH // HEAD_DIM
MOBA_BLOCK = 256
MOBA_TOPK = 3
Q_CHUNK = 128
ROPE_THETA = 500000.0
ROPE_DIM = HEAD_DIM // 4
ATTN_SCALE = HEAD_DIM ** -0.5
G_VAL_WIDTH = D_MODEL - A_WIDTH
G_HEADS = 4
G_VAL_DIM = G_VAL_WIDTH // G_HEADS
G_KEY_WIDTH = G_VAL_WIDTH // 2
G_KEY_DIM = G_KEY_WIDTH // G_HEADS
GATE_RANK = 16
GATE_TAU = 16.0
GLA_CHUNK = 64
MIX_WIDTH = A_WIDTH + G_VAL_WIDTH
IN_SIZES = [A_WIDTH, A_WIDTH, A_WIDTH, G_KEY_WIDTH, G_KEY_WIDTH, G_VAL_WIDTH, G_VAL_WIDTH, GATE_RANK]
IN_WIDTH = sum(IN_SIZES)
IN_SPLITS = np.cumsum(IN_SIZES)[:-1].tolist()
N_GROUPS = 4
EXPERTS_PER_GROUP = 8
N_EXPERTS = N_GROUPS * EXPERTS_PER_GROUP
TOP_K = 2
D_EXPERT = D_MODEL // 2
MOE_BLOCK = 128
EPS = 1e-6

kernel_name = 'hymba_moba_gla_hmoe_adaln'


def rms_norm(x, g):
    xf = x.astype(jnp.float32)
    y = xf * lax.rsqrt(jnp.mean(xf * xf, axis=-1, keepdims=True) + EPS)
    return (y * g.astype(jnp.float32)).astype(x.dtype)


def partial_rotary(x):
    S = x.shape[1]
    half = ROPE_DIM // 2
    inv = jnp.power(ROPE_THETA, -jnp.arange(half, dtype=jnp.float32) * (2.0 / ROPE_DIM))
    ang = jnp.arange(S, dtype=jnp.float32)[:, None] * inv[None, :]
    cos = jnp.cos(ang)[None, :, None, :]
    sin = jnp.sin(ang)[None, :, None, :]
    xr = x[..., :ROPE_DIM].astype(jnp.float32)
    x1, x2 = xr[..., :half], xr[..., half:]
    rot = jnp.concatenate([x1 * cos - x2 * sin, x2 * cos + x1 * sin], axis=-1).astype(x.dtype)
    return jnp.concatenate([rot, x[..., ROPE_DIM:]], axis=-1)


def moba_sequence(q, k, v):
    H, S, Dh = q.shape
    n_blk = max(-(-S // MOBA_BLOCK), MOBA_TOPK)
    pad = n_blk * MOBA_BLOCK - S
    k_p = jnp.pad(k, ((0, 0), (0, pad), (0, 0)))
    v_p = jnp.pad(v, ((0, 0), (0, pad), (0, 0)))
    kb = k_p.reshape(H, n_blk, MOBA_BLOCK, Dh)
    vb = v_p.reshape(H, n_blk, MOBA_BLOCK, Dh)
    kmean = jnp.mean(kb.astype(jnp.float32), axis=2).astype(k.dtype)
    n_chunks = S // Q_CHUNK

    def chunk(ci):
        q0 = ci * Q_CHUNK
        qc = lax.dynamic_slice_in_dim(q, q0, Q_CHUNK, axis=1)
        qpos = q0 + jnp.arange(Q_CHUNK)
        blk = q0 // MOBA_BLOCK
        gate = jnp.einsum('hqd,hnd->hqn', qc, kmean).astype(jnp.float32)
        past = jnp.arange(n_blk) < blk
        gate = jnp.where(past[None, None, :], gate, -jnp.inf)
        _, sel = lax.top_k(gate, MOBA_TOPK)
        valid = jnp.arange(MOBA_TOPK) < blk
        ksel = jax.vmap(lambda kh, ih: kh[ih])(kb, sel)
        vsel = jax.vmap(lambda vh, ih: vh[ih])(vb, sel)
        s_sel = jnp.einsum('hqd,hqkld->hqkl', qc, ksel).astype(jnp.float32)
        s_sel = jnp.where(valid[None, None, :, None], s_sel, -jnp.inf)
        s_sel = s_sel.reshape(H, Q_CHUNK, MOBA_TOPK * MOBA_BLOCK)
        kown = lax.dynamic_slice_in_dim(k_p, blk * MOBA_BLOCK, MOBA_BLOCK, axis=1)
        vown = lax.dynamic_slice_in_dim(v_p, blk * MOBA_BLOCK, MOBA_BLOCK, axis=1)
        kpos = blk * MOBA_BLOCK + jnp.arange(MOBA_BLOCK)
        s_own = jnp.einsum('hqd,hld->hql', qc, kown).astype(jnp.float32)
        s_own = jnp.where(kpos[None, None, :] <= qpos[None, :, None], s_own, -jnp.inf)
        p = jax.nn.softmax(jnp.concatenate([s_sel, s_own], axis=-1), axis=-1).astype(v.dtype)
        p_sel = p[..., :MOBA_TOPK * MOBA_BLOCK].reshape(H, Q_CHUNK, MOBA_TOPK, MOBA_BLOCK)
        p_own = p[..., MOBA_TOPK * MOBA_BLOCK:]
        return (jnp.einsum('hqkl,hqkld->hqd', p_sel, vsel)
                + jnp.einsum('hql,hld->hqd', p_own, vown))

    out = lax.map(chunk, jnp.arange(n_chunks))
    return out.transpose(1, 0, 2, 3).reshape(H, S, Dh)


def moba_attention(q, k, v):
    return lax.map(lambda a: moba_sequence(a[0], a[1], a[2]), (q, k, v))


def gla_chunked(q, k, v, log_a):
    B, S, H, dk = q.shape
    dv = v.shape[-1]
    nc = S // GLA_CHUNK

    def blocks(t):
        return t.reshape(B, nc, GLA_CHUNK, H, t.shape[-1]).transpose(0, 3, 1, 2, 4)

    q, k, v, la = blocks(q), blocks(k), blocks(v), blocks(log_a)
    b = jnp.cumsum(la, axis=3)
    b_last = b[:, :, :, -1:, :]
    q_dec = q * jnp.exp(b)
    k_inv = k * jnp.exp(-b)
    k_end = k * jnp.exp(b_last - b)
    causal = jnp.tril(jnp.ones((GLA_CHUNK, GLA_CHUNK), dtype=bool))
    att = jnp.einsum('bhnte,bhnse->bhnts', q_dec, k_inv)
    att = jnp.where(causal, att, 0.0)
    o_intra = jnp.einsum('bhnts,bhnsv->bhntv', att, v)
    kv = jnp.einsum('bhnse,bhnsv->bhnev', k_end, v)
    decay = jnp.exp(b_last[:, :, :, 0, :])

    def step(state, inp):
        d, kvc = inp
        return d[..., None] * state + kvc, state

    init = jnp.zeros((B, H, dk, dv), jnp.float32)
    _, s_prev = lax.scan(step, init, (decay.transpose(2, 0, 1, 3), kv.transpose(2, 0, 1, 3, 4)))
    s_prev = s_prev.transpose(1, 2, 0, 3, 4)
    o_inter = jnp.einsum('bhnte,bhnev->bhntv', q_dec, s_prev)
    return (o_intra + o_inter).transpose(0, 2, 3, 1, 4).reshape(B, S, H, dv)


def gla_mixer(q, k, v, og, ga, w_gk, b_gk, g_onorm):
    B, S, _ = q.shape
    f32 = jnp.float32
    q = q.reshape(B, S, G_HEADS, G_KEY_DIM).astype(f32) * (G_KEY_DIM ** -0.5)
    k = k.reshape(B, S, G_HEADS, G_KEY_DIM).astype(f32)
    v = v.reshape(B, S, G_HEADS, G_VAL_DIM).astype(f32)
    z = (ga @ w_gk + b_gk).astype(f32).reshape(B, S, G_HEADS, G_KEY_DIM)
    log_a = jax.nn.log_sigmoid(z) / GATE_TAU
    o = gla_chunked(q, k, v, log_a)
    o = o * lax.rsqrt(jnp.mean(o * o, axis=-1, keepdims=True) + EPS) * g_onorm.astype(f32)
    o = o * jax.nn.silu(og.reshape(B, S, G_HEADS, G_VAL_DIM).astype(f32))
    return o.reshape(B, S, G_VAL_WIDTH).astype(og.dtype)


def hier_moe(h, w_r1, b_r1, w_r2, b_r2, w_gate, w_up, w_down):
    B, S, D = h.shape
    T = B * S
    t = h.reshape(T, D)
    tf = t.astype(jnp.float32)
    l1 = tf @ w_r1.astype(jnp.float32) + b_r1.astype(jnp.float32)
    p1 = jax.nn.softmax(l1, axis=-1)
    grp = jnp.argmax(l1, axis=-1)
    p_grp = jnp.take_along_axis(p1, grp[:, None], axis=-1)
    l2 = (tf @ w_r2.astype(jnp.float32) + b_r2.astype(jnp.float32)).reshape(T, N_GROUPS, EXPERTS_PER_GROUP)
    l2g = jnp.take_along_axis(l2, grp[:, None, None], axis=1)[:, 0]
    v2, i2 = lax.top_k(l2g, TOP_K)
    p2 = jax.nn.softmax(v2, axis=-1)
    weights = (p_grp * p2).reshape(-1)
    experts = (grp[:, None] * EXPERTS_PER_GROUP + i2).reshape(-1).astype(jnp.int32)
    tokens = jnp.repeat(jnp.arange(T, dtype=jnp.int32), TOP_K)
    N = T * TOP_K
    counts = jnp.zeros((N_EXPERTS,), jnp.int32).at[experts].add(1)
    padded = (counts + MOE_BLOCK - 1) // MOE_BLOCK * MOE_BLOCK
    ends = jnp.cumsum(padded)
    starts = ends - padded
    cstart = jnp.cumsum(counts) - counts
    order = jnp.argsort(experts)
    e_sorted = experts[order]
    rank = jnp.arange(N, dtype=jnp.int32) - cstart[e_sorted]
    dest = starts[e_sorted] + rank
    P = -(-N // MOE_BLOCK) * MOE_BLOCK + N_EXPERTS * MOE_BLOCK
    n_blocks = P // MOE_BLOCK
    slot_tok = jnp.full((P,), T, jnp.int32).at[dest].set(tokens[order])
    slot_w = jnp.zeros((P,), h.dtype).at[dest].set(weights[order].astype(h.dtype))
    block_exp = jnp.minimum(jnp.searchsorted(ends, jnp.arange(n_blocks) * MOE_BLOCK, side='right'),
                            N_EXPERTS - 1).astype(jnp.int32)
    t_pad = jnp.concatenate([t, jnp.zeros((1, D), t.dtype)], axis=0)
    xb = t_pad[slot_tok].reshape(n_blocks, MOE_BLOCK, D)

    def run(args):
        xblk, e = args
        return (jax.nn.silu(xblk @ w_gate[e]) * (xblk @ w_up[e])) @ w_down[e]

    yb = lax.map(run, (xb, block_exp)).reshape(P, D)
    y = jax.ops.segment_sum(yb * slot_w[:, None], slot_tok, num_segments=T + 1)[:T]
    return y.reshape(B, S, D)


def setup_inputs(seed: int = 0) -> dict:
    key = jax.random.key(seed)
    ks = jax.random.split(key, 20)
    f32 = jnp.float32

    def nrm(k, shape, scale):
        return jax.random.normal(k, shape, f32) * scale

    return {
        'x': nrm(ks[0], (BATCH, SEQ, D_MODEL), 1.0),
        'c': nrm(ks[1], (BATCH, D_MODEL), 1.0),
        'ln1': 1.0 + nrm(ks[2], (DEPTH, D_MODEL), 0.02),
        'ln2': 1.0 + nrm(ks[3], (DEPTH, D_MODEL), 0.02),
        'w_ada': nrm(ks[4], (DEPTH, D_MODEL, 6 * D_MODEL), 0.5 * D_MODEL ** -0.5),
        'b_ada': nrm(ks[5], (DEPTH, 6 * D_MODEL), 0.02),
        'w_in': nrm(ks[6], (DEPTH, D_MODEL, IN_WIDTH), D_MODEL ** -0.5),
        'w_gk': nrm(ks[7], (DEPTH, GATE_RANK, G_KEY_WIDTH), GATE_RANK ** -0.5),
        'b_gk': nrm(ks[8], (DEPTH, G_KEY_WIDTH), 0.1),
        'g_onorm': 1.0 + nrm(ks[9], (DEPTH, G_VAL_DIM), 0.02),
        'w_out': nrm(ks[10], (DEPTH, MIX_WIDTH, D_MODEL), MIX_WIDTH ** -0.5),
        'w_r1': nrm(ks[11], (DEPTH, D_MODEL, N_GROUPS), D_MODEL ** -0.5),
        'b_r1': nrm(ks[12], (DEPTH, N_GROUPS), 0.01),
        'w_r2': nrm(ks[13], (DEPTH, D_MODEL, N_EXPERTS), D_MODEL ** -0.5),
        'b_r2': nrm(ks[14], (DEPTH, N_EXPERTS), 0.01),
        'w_e_gate': nrm(ks[15], (DEPTH, N_EXPERTS, D_MODEL, D_EXPERT), D_MODEL ** -0.5),
        'w_e_up': nrm(ks[16], (DEPTH, N_EXPERTS, D_MODEL, D_EXPERT), D_MODEL ** -0.5),
        'w_e_down': nrm(ks[17], (DEPTH, N_EXPERTS, D_EXPERT, D_MODEL), D_EXPERT ** -0.5),
        'ln_f': 1.0 + nrm(ks[18], (D_MODEL,), 0.02),
    }


def reference(x, c, ln1, ln2, w_ada, b_ada, w_in, w_gk, b_gk, g_onorm, w_out,
              w_r1, b_r1, w_r2, b_r2, w_e_gate, w_e_up, w_e_down, ln_f):
    B, S, D = x.shape
    c_act = jax.nn.silu(c)
    for l in range(DEPTH):
        mod = (c_act @ w_ada[l] + b_ada[l])[:, None, :]
        sh1, sc1, gt1, sh2, sc2, gt2 = jnp.split(mod, 6, axis=-1)
        h = rms_norm(x, ln1[l]) * (1.0 + sc1) + sh1
        proj = h @ w_in[l]
        qa, ka, va, qg, kg, vg, og, ga = jnp.split(proj, IN_SPLITS, axis=-1)
        qa = partial_rotary(qa.reshape(B, S, A_HEADS, HEAD_DIM)) * ATTN_SCALE
        ka = partial_rotary(ka.reshape(B, S, A_HEADS, HEAD_DIM))
        va = va.reshape(B, S, A_HEADS, HEAD_DIM)
        oa = moba_attention(qa.transpose(0, 2, 1, 3), ka.transpose(0, 2, 1, 3), va.transpose(0, 2, 1, 3))
        oa = oa.transpose(0, 2, 1, 3).reshape(B, S, A_WIDTH)
        ob = gla_mixer(qg, kg, vg, og, ga, w_gk[l], b_gk[l], g_onorm[l])
        mix = jnp.concatenate([oa, ob], axis=-1) @ w_out[l]
        x = x + gt1 * mix
        h2 = rms_norm(x, ln2[l]) * (1.0 + sc2) + sh2
        x = x + gt2 * hier_moe(h2, w_r1[l], b_r1[l], w_r2[l], b_r2[l], w_e_gate[l], w_e_up[l], w_e_down[l])
    return rms_norm(x, ln_f)
```

```python
import numpy as np
import concourse.bass as bass
import concourse.mybir as mybir
from concourse.bass_utils import run_bass_kernel_spmd

F32 = mybir.dt.float32
BF16 = mybir.dt.bfloat16
I32 = mybir.dt.int32
AF = mybir.ActivationFunctionType
ALU = mybir.AluOpType
AX = mybir.AxisListType

NCORES = 8
S_ = 2048
D_ = 2048
NT = 16
KT = 16
DEPTH = 2
INW = 6160
CAP = 512
NEXP = 32
NSLOT = NEXP * CAP
EPS = 1e-6
ATTN_SCALE = 128 ** -0.5
GK_SCALE = 128 ** -0.5
NEG = -1.0e30
DBG = {'skip_c1': False, 'gla_steps': 99, 'gla_tiles': NT, 'gla_heads': 4}


class Sched:
    def __init__(self, nc):
        self.nc = nc
        self.engs = {'pe': nc.tensor, 'act': nc.scalar, 'dve': nc.vector, 'pool': nc.gpsimd, 'sp': nc.sync}
        self.esem = {e: nc.alloc_semaphore('c_' + e) for e in ['pe', 'act', 'dve', 'pool']}
        self.ecnt = {e: 0 for e in self.esem}
        self.known = {e: {} for e in self.engs}
        self.res = {}
        self.dsems = {}
        self.nwait = 0

    def _wait(self, eng, ev):
        key, sem, val = ev
        if self.known[eng].get(key, 0) >= val:
            return
        self.engs[eng].wait_ge(sem, val)
        self.known[eng][key] = val
        self.nwait += 1

    def _deps(self, eng, reads, writes):
        best = {}

        def add(ev):
            if ev[0] not in best or best[ev[0]][2] < ev[2]:
                best[ev[0]] = ev
        for r in reads:
            st = self.res.get(r)
            if st:
                for ev in st['w'].values():
                    if not (ev[0] == eng and eng == 'pe'):
                        add(ev)
                if r.startswith('ps:'):
                    for ev in st['r'].values():
                        if ev[0] != eng:
                            add(ev)
        for w in writes:
            st = self.res.get(w)
            if st:
                for ev in st['w'].values():
                    if ev[0] != eng:
                        add(ev)
                for ev in st['r'].values():
                    if ev[0] != eng:
                        add(ev)
        for ev in best.values():
            self._wait(eng, ev)

    def _record(self, ev, reads, writes, acc=False):
        for r in reads:
            st = self.res.setdefault(r, {'w': {}, 'r': {}})
            st['r'][ev[0]] = ev
        for w in writes:
            if acc and w in self.res:
                self.res[w]['w'][ev[0]] = ev
                self.res[w]['r'] = {}
            else:
                self.res[w] = {'w': {ev[0]: ev}, 'r': {}}

    def op(self, eng, fn, reads=(), writes=()):
        self._deps(eng, reads, writes)
        ins = fn(self.engs[eng])
        self.ecnt[eng] += 1
        ins.then_inc(self.esem[eng], 1)
        self._record((eng, self.esem[eng], self.ecnt[eng]), reads, writes)

    def group(self, eng, fns, reads=(), writes=()):
        self._deps(eng, reads, writes)
        ins = None
        for fn in fns:
            ins = fn(self.engs[eng])
        self.ecnt[eng] += 1
        ins.then_inc(self.esem[eng], 1)
        self._record((eng, self.esem[eng], self.ecnt[eng]), reads, writes)

    def dma(self, eng, fn, reads=(), writes=(), slot=None, acc=False):
        self._deps(eng, reads, writes)
        if slot is None:
            slot = writes[0] if writes[0].startswith('sb:') else 'st_' + reads[0]
        if slot not in self.dsems:
            self.dsems[slot] = [self.nc.alloc_semaphore('d%d' % len(self.dsems)), 0]
        sc = self.dsems[slot]
        sc[1] += 16
        fn(self.engs[eng]).then_inc(sc[0], 16)
        self._record(('d_' + slot, sc[0], sc[1]), reads, writes, acc=acc)

    def barrier(self):
        evs = [(e, self.esem[e], self.ecnt[e]) for e in self.esem if self.ecnt[e] > 0]
        evs += [('d_' + s, sc[0], sc[1]) for s, sc in self.dsems.items()]
        for eng in self.engs:
            for ev in evs:
                if ev[0] != eng:
                    self._wait(eng, ev)
        self.res = {}

    def finish(self):
        evs = [(e, self.esem[e], self.ecnt[e]) for e in self.esem if self.ecnt[e] > 0]
        evs += [('d_' + s, sc[0], sc[1]) for s, sc in self.dsems.items()]
        for ev in evs:
            self._wait('sp', ev)


def build_nc(n_layers=DEPTH, stop_after=None, debug=False):
    nc = bass.Bass("TRN2", target_bir_lowering=False)
    S = Sched(nc)

    def din(name, shape, dt=F32):
        return nc.dram_tensor(name, list(shape), dt, kind="ExternalInput").ap()

    x_in = din("x", [S_, D_])
    cT_in = din("cT", [128, KT])
    lnT_in = din("lnT", [128, 2 * DEPTH + 1, KT])
    badaT_in = din("badaT", [128, DEPTH, 96])
    w_ada = din("w_ada", [DEPTH, D_, 6 * D_])
    w_in = din("w_in", [DEPTH, D_, INW])
    wgk_in = din("wgk_aug", [17, DEPTH, 512])
    gon_in = din("gon_b", [128, DEPTH, 256])
    early = ('mod', 'B', 'C1', 'C2')
    w_out = din("w_out", [DEPTH, D_, D_]) if stop_after not in early else None
    wr_in = din("wr", [128, DEPTH, KT, 36])
    br_in = din("br", [1, DEPTH, 36])
    if stop_after not in early + ('D',):
        w_eg = din("w_e_gate", [DEPTH, NEXP, D_, 1024])
        w_eu = din("w_e_up", [DEPTH, NEXP, D_, 1024])
        w_ed = din("w_e_down", [DEPTH, NEXP, 1024, D_])
    rope_in = din("rope", [128, NT, 2, 2, 32])
    consts_in = din("consts", [128, 6, 128])
    negm_in = din("negmask", [128, 8, 8])
    ebase_in = din("ebase", [128, NEXP])
    y_out = nc.dram_tensor("y", [S_, D_], F32, kind="ExternalOutput").ap()
    dbg = {}

    def dbg_out(name, shape, dt=F32):
        dbg[name] = nc.dram_tensor(name, list(shape), dt, kind="ExternalOutput").ap()
        return dbg[name]

    xd = nc.dram_tensor("xd", [S_, D_], F32).ap()
    mixTd = nc.dram_tensor("mixTd", [KT, 128, S_], BF16).ap()
    Xg = nc.dram_tensor("Xg", [NSLOT, D_], BF16).ap()
    Yg = nc.dram_tensor("Yg", [NSLOT, D_], F32).ap()

    sb = nc.alloc_sbuf_tensor
    _uid = [0]

    def SBT(name, shape, dt):
        _uid[0] += 1
        return nc.sbuf_tensor("%s_u%d" % (name, _uid[0]), shape, dt)
    cst_f = sb("cst_f", [128, 6, 128], F32)
    cst_b = sb("cst_b", [128, 6, 128], BF16)
    ident_f, triI_f, triR_f, triS_f, ones_f = (cst_f[:, i, :] for i in range(5))
    ident_b, triI_b = cst_b[:, 0, :], cst_b[:, 1, :]
    rope = sb("rope_sb", [128, NT, 2, 2, 32], F32)
    negm = sb("negm", [128, 8, 8], F32)
    ebase = sb("ebase_sb", [128, NEXP], F32)
    lnT = sb("lnT_sb", [128, 2 * DEPTH + 1, KT], F32)
    modT = sb("modT", [128, DEPTH, 96], F32)
    g1T = sb("g1T", [128, KT], F32)
    g2T = sb("g2T", [128, KT], F32)
    wgk = sb("wgk", [17, DEPTH, 512], F32)
    gon = sb("gon", [128, DEPTH, 256], F32)
    wr = sb("wr_sb", [128, DEPTH, KT, 36], F32)
    br = sb("br_sb", [1, DEPTH, 36], F32)
    idx_a = sb("idx_a", [128, NT], I32)
    idx_b = sb("idx_b", [128, NT], I32)
    wts = sb("wts", [128, NT, 2], F32)
    selacc = sb("selacc", [128, NEXP], F32)
    sm = sb("sm", [128, 64], F32)
    cact = sb("cact", [128, KT], BF16)
    badaT = sb("badaT_sb", [128, DEPTH, 96], F32)
    ps = [nc.alloc_psum_tensor("ps%d" % i, [128, 512], F32) for i in range(8)]
    P = ['ps:%d' % i for i in range(8)]

    def smc(i, n=1):
        return sm[:, i:i + n]

    def ld(dst, src, name):
        S.dma('sp', lambda e: e.dma_start(out=dst, in_=src), reads=['dr:' + name], writes=['sb:' + name])
    ld(cst_f[:, :, :], consts_in[:, :, :], 'cst_f')
    ld(rope[:, :, :, :, :], rope_in[:, :, :, :, :], 'rope')
    ld(negm[:, :, :], negm_in[:, :, :], 'negm')
    ld(ebase[:, :], ebase_in[:, :], 'ebase')
    ld(lnT[:, :, :], lnT_in[:, :, :], 'lnT')
    ld(wgk[:, :, :], wgk_in[:, :, :], 'wgk')
    ld(gon[:, :, :], gon_in[:, :, :], 'gon')
    ld(wr[:, :, :, :], wr_in[:, :, :, :], 'wr')
    ld(br[:, :, :], br_in[:, :, :], 'br')
    S.op('dve', lambda e: e.tensor_copy(out=cst_b[:, :, :], in_=cst_f[:, :, :]), reads=['sb:cst_f'], writes=['sb:cst_b'])

    import contextlib
    with contextlib.ExitStack() as es:
        cT = es.enter_context(SBT("cT_sb", [128, KT], F32))
        wring = [es.enter_context(SBT("wa%d" % i, [128, KT, 512], BF16)) for i in range(3)]
        ld(cT[:, :], cT_in[:, :], 'cT')
        ld(badaT[:, :, :], badaT_in[:, :, :], 'badaT')
        S.op('act', lambda e: e.activation(out=cact[:, :], in_=cT[:, :], func=AF.Silu), reads=['sb:cT'], writes=['sb:cact'])
        for l in range(1):
            wv = w_ada[l].rearrange("(k p) n -> p k n", p=128)
            for pc in range(24):
                buf = wring[pc % 3]
                rn = 'sb:wa%d' % (pc % 3)
                for hh in range(2):
                    S.dma('pool', lambda e, buf=buf, pc=pc, hh=hh: e.dma_start(
                        out=buf[:, hh * 8:(hh + 1) * 8, :], in_=wv[:, hh * 8:(hh + 1) * 8, pc * 512:(pc + 1) * 512]),
                        reads=['dr:w_ada'], writes=[rn], acc=(hh == 1))
                for m in range(4):
                    j = pc * 4 + m
                    bank = (l * 96 + j) // 512
                    col = l * 96 + j
                    S.group('pe', [lambda e, buf=buf, m=m, k=k, col=col: e.matmul(
                        ps[0][:, col:col + 1], lhsT=buf[:, k, m * 128:(m + 1) * 128], rhs=cact[:, k:k + 1],
                        start=(k == 0), stop=(k == KT - 1)) for k in range(KT)],
                        reads=[rn, 'sb:cact'], writes=[P[0]])
        S.op('dve', lambda e: e.tensor_tensor(out=modT[:, 0, :], in0=ps[0][:, 0:96], in1=badaT[:, 0, :], op=ALU.add),
             reads=[P[0], 'sb:badaT'], writes=['sb:modT'])
        S.barrier()
    if debug:
        d = dbg_out("dbg_modT", [128, DEPTH, 96])
        S.dma('sp', lambda e: e.dma_start(out=d[:, :, :], in_=modT[:, :, :]), reads=['sb:modT'], writes=['dr:dbg_modT'])
    if stop_after == 'mod':
        S.finish()
        return nc, dbg

    def bcast(dst, vcols, dg, rname, vres, pbank=7):
        for kt in range(KT):
            S.op('dve', lambda e, kt=kt: e.tensor_scalar(out=dg[:, :], in0=ident_f, scalar1=vcols(kt), scalar2=None, op0=ALU.mult),
                 reads=['sb:cst_f'] + vres, writes=['sb:dg'])
            S.op('pe', lambda e, kt=kt: e.matmul(ps[pbank][:, 0:128], lhsT=ones_f, rhs=dg[:, :], start=True, stop=True),
                 reads=['sb:dg', 'sb:cst_f'], writes=[P[pbank]])
            S.op('act', lambda e, kt=kt: e.activation(out=dst[:, kt * 128:(kt + 1) * 128], in_=ps[pbank][:, 0:128], func=AF.Copy),
                 reads=[P[pbank]], writes=[rname])

    def rstd_from_ss(ss_col, out_col, n, tag):
        S.op('act', lambda e: e.activation(out=out_col, in_=ss_col, func=AF.Ln, scale=1.0 / n, bias=eps_t[:, 0:1]),
             reads=['sb:' + tag + '_ss', 'sb:eps'], writes=['sb:' + tag + '_sq'])
        S.op('act', lambda e: e.activation(out=out_col, in_=out_col, func=AF.Exp, scale=-0.5), reads=['sb:' + tag + '_sq'], writes=['sb:' + tag + '_rs'])

    eps_t = sb("eps_t", [128, 1], F32)
    bc_reg = nc.gpsimd.to_reg(NSLOT - 1)
    S.op('pool', lambda e: e.memset(eps_t[:, :], EPS), writes=['sb:eps'])

    for l in range(n_layers):
        x_src = x_in if l == 0 else xd
        xres = 'dr:x_in' if l == 0 else 'dr:xd'
        last = (l == n_layers - 1)
        sh1 = lambda kt, l=l: modT[:, l, 0 + kt:0 + kt + 1]
        sc1 = lambda l=l: modT[:, l, 16:32]
        gt1 = lambda kt, l=l: modT[:, l, 32 + kt:32 + kt + 1]
        sh2 = lambda kt, l=l: modT[:, l, 48 + kt:48 + kt + 1]
        sc2 = lambda l=l: modT[:, l, 64:80]
        gt2 = lambda kt, l=l: modT[:, l, 80 + kt:80 + kt + 1]
        S.op('dve', lambda e: e.scalar_tensor_tensor(out=g1T[:, :], in0=sc1(), scalar=1.0, in1=lnT[:, 2 * l, :], op0=ALU.add, op1=ALU.mult),
             reads=['sb:modT', 'sb:lnT'], writes=['sb:g1T'])
        S.op('dve', lambda e: e.scalar_tensor_tensor(out=g2T[:, :], in0=sc2(), scalar=1.0, in1=lnT[:, 2 * l + 1, :], op0=ALU.add, op1=ALU.mult),
             reads=['sb:modT', 'sb:lnT'], writes=['sb:g2T'])

        with contextlib.ExitStack() as esL:
            hT = esL.enter_context(SBT("hT", [128, KT, S_], BF16))
            with contextlib.ExitStack() as es:
                xt = [es.enter_context(SBT("xt%d" % i, [128, D_], F32)) for i in range(2)]
                junk = es.enter_context(SBT("junk", [128, D_], BF16))
                for i in range(NT):
                    xb = xt[i % 2]
                    xn_ = 'sb:xt%d' % (i % 2)
                    S.dma('sp', lambda e, xb=xb, i=i: e.dma_start(out=xb[:, :], in_=x_src[i * 128:(i + 1) * 128, :]),
                          reads=[xres], writes=[xn_])
                    S.op('act', lambda e, xb=xb: e.activation(out=junk[:, :], in_=xb[:, :], func=AF.Square, accum_out=smc(0)),
                         reads=[xn_], writes=['sb:junk', 'sb:n1_ss'])
                    rstd_from_ss(smc(0), smc(1), D_, 'n1')
                    S.op('act', lambda e, xb=xb: e.activation(out=xb[:, :], in_=xb[:, :], func=AF.Copy, scale=smc(1)),
                         reads=[xn_, 'sb:n1_rs'], writes=[xn_])
                    for g4 in range(4):
                        bank = g4 % 4
                        S.group('pe', [lambda e, xb=xb, kt=kt, bank=bank: e.transpose(
                            ps[bank][:, (kt % 4) * 128:(kt % 4 + 1) * 128], xb[:, kt * 128:(kt + 1) * 128], ident_f)
                            for kt in range(g4 * 4, g4 * 4 + 4)], reads=[xn_, 'sb:cst_f'], writes=[P[bank]])
                        for kt in range(g4 * 4, g4 * 4 + 4):
                            eng = 'dve' if kt % 2 == 0 else 'dve'
                            S.op(eng, lambda e, kt=kt, bank=bank, i=i: e.tensor_scalar(
                                out=hT[:, kt, i * 128:(i + 1) * 128], in0=ps[bank][:, (kt % 4) * 128:(kt % 4 + 1) * 128],
                                scalar1=g1T[:, kt:kt + 1], scalar2=sh1(kt), op0=ALU.mult, op1=ALU.add),
                                reads=[P[bank], 'sb:g1T', 'sb:modT'], writes=['sb:hT'])
                S.barrier()
            if debug and l == 0:
                d = dbg_out("dbg_hT", [KT, 128, S_], BF16)
                S.dma('sp', lambda e: e.dma_start(out=d.rearrange("k p s -> p k s"), in_=hT[:, :, :]), reads=['sb:hT'], writes=['dr:dbg_hT'])
            if stop_after == 'B':
                S.finish()
                return nc, dbg

            wl = w_in[l].rearrange("(k p) n -> p k n", p=128)
            with contextlib.ExitStack() as es:
                wA = [es.enter_context(SBT("wA%d" % i, [128, KT, 384], BF16)) for i in range(2)]
                tA2 = [es.enter_context(SBT("tA%d" % z, [128, 2, 32], F32)) for z in range(2)]
                tB2 = [es.enter_context(SBT("tB%d" % z, [128, 2, 32], F32)) for z in range(2)]
                qkb2 = [es.enter_context(SBT("qkb%d" % z, [128, 2, 128], BF16)) for z in range(2)]
                qkT = es.enter_context(SBT("qkT", [128, 2, S_], BF16))
                vS = es.enter_context(SBT("vS", [128, NT, 132], BF16))
                kmf = es.enter_context(SBT("kmf", [128, 8], F32))
                kmb = es.enter_context(SBT("kmb", [128, 8], BF16))
                gm = es.enter_context(SBT("gm", [128, 8], F32))
                top8 = es.enter_context(SBT("top8", [128, 8], F32))
                sel = es.enter_context(SBT("sel", [128, NT, 8], F32))
                pT = [es.enter_context(SBT("pT%d" % i, [128, 512], BF16)) for i in range(4)]
                oacc = es.enter_context(SBT("oacc", [128, NT, 132], F32))
                orec = es.enter_context(SBT("orec", [128, NT], F32))
                oab = [es.enter_context(SBT("oab%d" % i, [128, 128], BF16)) for i in range(2)]
                oT = [es.enter_context(SBT("oT%d" % i, [128, S_], BF16)) for i in range(2)]
                nxt = (l + 1 < n_layers)
                if nxt:
                    wring2 = [es.enter_context(SBT("wb%d" % i, [128, KT, 512], BF16)) for i in range(3)]
                    wv2 = w_ada[l + 1].rearrange("(k p) n -> p k n", p=128)

                def ada_piece(pc):
                    bufa = wring2[pc % 3]
                    rn = 'sb:wb%d' % (pc % 3)
                    for hh in range(2):
                        S.dma('pool', lambda e, hh=hh: e.dma_start(
                            out=bufa[:, hh * 8:(hh + 1) * 8, :], in_=wv2[:, hh * 8:(hh + 1) * 8, pc * 512:(pc + 1) * 512]),
                            reads=['dr:w_ada'], writes=[rn], acc=(hh == 1))
                    for m in range(4):
                        S.group('pe', [lambda e, m=m, k=k: e.matmul(
                            ps[3][:, 100 + m:101 + m], lhsT=bufa[:, k, m * 128:(m + 1) * 128], rhs=cact[:, k:k + 1],
                            start=(k == 0), stop=(k == KT - 1)) for k in range(KT)],
                            reads=[rn, 'sb:cact'], writes=[P[3]])
                    S.op('dve', lambda e: e.tensor_tensor(out=modT[:, l + 1, 4 * pc:4 * pc + 4], in0=ps[3][:, 100:104], in1=badaT[:, l + 1, 4 * pc:4 * pc + 4], op=ALU.add),
                         reads=[P[3], 'sb:badaT'], writes=['sb:modT_next'])
                if l == 0:
                    zer = es.enter_context(SBT("zer", [128, 2, D_], BF16))
                    S.op('pool', lambda e: e.memset(zer[:, :, :], 0.0), writes=['sb:zer'])
                    Xgv = Xg.rearrange("(n p) d -> p n d", p=128)
                    for z in range(NSLOT // 256):
                        S.dma('sp', lambda e, z=z: e.dma_start(out=Xgv[:, 2 * z:2 * z + 2, :], in_=zer[:, :, :]), reads=['sb:zer'], writes=['dr:Xg'], acc=True)
                S.op('pool', lambda e: e.memset(vS[:, :, :], 1.0), writes=['sb:vS'])
                S.op('pool', lambda e: e.memset(sel[:, :, :], 1.0), writes=['sb:sel'])

                def load_wA(h):
                    buf = wA[h % 2]
                    for j, c0 in enumerate([h * 128, 1024 + h * 128, 2048 + h * 128]):
                        S.dma('pool', lambda e, buf=buf, j=j, c0=c0: e.dma_start(
                            out=buf[:, :, j * 128:(j + 1) * 128], in_=wl[:, :, c0:c0 + 128]),
                            reads=['dr:w_in'], writes=['sb:wA%d' % (h % 2)], acc=(j > 0))
                if not DBG['skip_c1']:
                    load_wA(0)
                for h in range(0 if DBG['skip_c1'] else 8):
                    if h + 1 < 8:
                        load_wA(h + 1)
                    buf = wA[h % 2]
                    wn = 'sb:wA%d' % (h % 2)
                    def c1_mm(i):
                        pb = i % 2
                        S.group('pe', [lambda e, k=k, i=i, pb=pb: e.matmul(
                            ps[pb][:, 0:384], lhsT=hT[:, k, i * 128:(i + 1) * 128], rhs=buf[:, k, :],
                            start=(k == 0), stop=(k == KT - 1)) for k in range(KT)],
                            reads=['sb:hT', wn], writes=[P[pb]])

                    def c1_chain(i):
                        tA, tB, qkb = tA2[i % 2], tB2[i % 2], qkb2[i % 2]
                        pz = str(i % 2)
                        pb = i % 2
                        psv = ps[pb][:, 0:256].rearrange("p (a d) -> p a d", a=2)
                        cs = rope[:, i, 0, :, :]
                        sn = rope[:, i, 1, :, :]
                        S.op('dve', lambda e: e.tensor_tensor(out=tA[:, :, :], in0=psv[:, :, 0:32], in1=cs, op=ALU.mult),
                             reads=[P[pb], 'sb:rope'], writes=['sb:tA' + pz])
                        S.op('dve', lambda e: e.tensor_tensor(out=tB[:, :, 0:16], in0=psv[:, :, 16:32], in1=sn[:, :, 0:16], op=ALU.mult),
                             reads=[P[pb], 'sb:rope'], writes=['sb:tB' + pz])
                        S.op('dve', lambda e: e.tensor_tensor(out=tB[:, :, 16:32], in0=psv[:, :, 0:16], in1=sn[:, :, 16:32], op=ALU.mult),
                             reads=[P[pb], 'sb:rope'], writes=['sb:tB' + pz])
                        S.op('act', lambda e: e.activation(out=qkb[:, 0, 32:128], in_=ps[pb][:, 32:128], func=AF.Copy, scale=ATTN_SCALE),
                             reads=[P[pb]], writes=['sb:qkb' + pz])
                        S.op('act', lambda e: e.activation(out=qkb[:, 1, 32:128], in_=ps[pb][:, 160:256], func=AF.Copy),
                             reads=[P[pb]], writes=['sb:qkb' + pz])
                        S.op('act', lambda e: e.activation(out=vS[:, i, 0:128], in_=ps[pb][:, 256:384], func=AF.Copy),
                             reads=[P[pb]], writes=['sb:vS'])
                        S.op('dve', lambda e: e.tensor_tensor(out=qkb[:, :, 0:32], in0=tA[:, :, :], in1=tB[:, :, :], op=ALU.add),
                             reads=['sb:tA' + pz, 'sb:tB' + pz], writes=['sb:qkb' + pz])

                    def c1_tr(i):
                        qkb = qkb2[i % 2]
                        pz = str(i % 2)
                        tps = ps[2][:, 0:128].bitcast(BF16)
                        S.group('pe', [lambda e, a=a: e.transpose(tps[:, a * 128:(a + 1) * 128], qkb[:, a, :], ident_b) for a in range(2)],
                                reads=['sb:qkb' + pz, 'sb:cst_b'], writes=[P[2]])
                        S.op('dve', lambda e: e.tensor_copy(out=qkT[:, :, i * 128:(i + 1) * 128],
                                                            in_=tps.rearrange("p (a t) -> p a t", a=2)),
                             reads=[P[2]], writes=['sb:qkT'])
                    c1_mm(0)
                    for i in range(NT):
                        c1_chain(i)
                        if i + 1 < NT:
                            c1_mm(i + 1)
                        c1_tr(i)
                        if nxt and i in (3, 8, 13):
                            ada_piece(3 * h + (i - 3) // 5)
                    S.op('dve', lambda e: e.tensor_reduce(out=kmf[:, :], in_=qkT[:, 1, :].rearrange("p (n s) -> p n s", n=8), axis=AX.X, op=ALU.add),
                         reads=['sb:qkT'], writes=['sb:kmf'])
                    S.op('act', lambda e: e.activation(out=kmb[:, :], in_=kmf[:, :], func=AF.Copy, scale=1.0 / 256.0),
                         reads=['sb:kmf'], writes=['sb:kmb'])
                    for c in range(8, NT):
                        blk = c // 2
                        S.op('pe', lambda e, c=c: e.matmul(ps[3][:, 0:8], lhsT=qkT[:, 0, c * 128:(c + 1) * 128], rhs=kmb[:, :], start=True, stop=True),
                             reads=['sb:qkT', 'sb:kmb'], writes=[P[3]])
                        S.op('dve', lambda e, blk=blk: e.tensor_tensor(out=gm[:, :], in0=ps[3][:, 0:8], in1=negm[:, blk, :], op=ALU.add),
                             reads=[P[3], 'sb:negm'], writes=['sb:gm'])
                        S.op('dve', lambda e: e.max(out=top8[:, :], in_=gm[:, :]), reads=['sb:gm'], writes=['sb:top8'])
                        S.op('dve', lambda e, c=c: e.tensor_scalar(out=sel[:, c, :], in0=gm[:, :], scalar1=top8[:, 2:3], scalar2=None, op0=ALU.is_ge),
                             reads=['sb:gm', 'sb:top8'], writes=['sb:sel'])
                    ob_ = oT[h % 2]
                    on_ = 'sb:oT%d' % (h % 2)
                    S.op('pool', lambda e: e.memset(oacc[:, :, :], 0.0), writes=['sb:oacc'])
                    its = []
                    for b in range(8):
                        for s4 in range((2 * b) // 4, 4):
                            its.append((b, s4))
                    ocnt = [0]

                    def emit_scores(ii):
                        b, s4 = its[ii]
                        info = []
                        for jj in range(2):
                            j = 2 * b + jj
                            c0 = max(4 * s4, j)
                            nq = 4 * s4 + 4 - c0
                            if nq <= 0:
                                info.append(None)
                                continue
                            sbk = 4 + 2 * (ii % 2) + jj
                            pt = pT[2 * (ii % 2) + jj]
                            pn = 'sb:pT%d' % (2 * (ii % 2) + jj)
                            S.op('pe', lambda e, j=j, c0=c0, nq=nq, sbk=sbk: e.matmul(
                                ps[sbk][:, 0:nq * 128], lhsT=qkT[:, 1, j * 128:(j + 1) * 128], rhs=qkT[:, 0, c0 * 128:(c0 + nq) * 128],
                                start=True, stop=True), reads=['sb:qkT'], writes=[P[sbk]])
                            S.op('act', lambda e, sbk=sbk, nq=nq, pt=pt: e.activation(out=pt[:, 0:nq * 128], in_=ps[sbk][:, 0:nq * 128], func=AF.Exp),
                                 reads=[P[sbk]], writes=[pn])
                            if c0 == j:
                                S.op('pool', lambda e, pt=pt: e.tensor_tensor(out=pt[:, 0:128], in0=pt[:, 0:128], in1=triI_b, op=ALU.mult),
                                     reads=[pn, 'sb:cst_b'], writes=[pn])
                            info.append((j, c0, nq, pt, pn))
                        return info

                    def emit_pv(ii, info):
                        b, s4 = its[ii]
                        for c in range(max(4 * s4, 2 * b), 4 * s4 + 4):
                            own = (c // 2 == b)
                            parts = []
                            for t in info:
                                if t is None:
                                    continue
                                j, c0, nq, pt, pn = t
                                if c < c0 or j > c:
                                    continue
                                parts.append((j, pt, pn, (c - c0) * 128))
                            if not parts:
                                continue
                            ob_i = ocnt[0] % 4
                            ocnt[0] += 1
                            obk = ob_i
                            oc0 = 0
                            S.group('pe', [lambda e, j=j, pt=pt, off=off, obk=obk, oc0=oc0, first=(pi_ == 0), lastp=(pi_ == len(parts) - 1): e.matmul(
                                ps[obk][:, oc0:oc0 + 129], lhsT=pt[:, off:off + 128], rhs=vS[:, j, 0:129], start=first, stop=lastp)
                                for pi_, (j, pt, pn, off) in enumerate(parts)], reads=[p[2] for p in parts] + ['sb:vS'], writes=[P[obk]])
                            if own:
                                S.op('dve', lambda e, obk=obk, oc0=oc0, c=c: e.tensor_tensor(
                                    out=oacc[:, c, 0:129], in0=ps[obk][:, oc0:oc0 + 129], in1=oacc[:, c, 0:129], op=ALU.add),
                                    reads=[P[obk], 'sb:oacc'], writes=['sb:oacc'])
                            else:
                                S.op('dve', lambda e, obk=obk, oc0=oc0, c=c, b=b: e.scalar_tensor_tensor(
                                    out=oacc[:, c, 0:129], in0=ps[obk][:, oc0:oc0 + 129], scalar=sel[:, c, b:b + 1], in1=oacc[:, c, 0:129],
                                    op0=ALU.mult, op1=ALU.add), reads=[P[obk], 'sb:sel', 'sb:oacc'], writes=['sb:oacc'])
                    prev = None
                    for ii in range(len(its)):
                        inf = emit_scores(ii)
                        if prev is not None:
                            emit_pv(*prev)
                        prev = (ii, inf)
                    emit_pv(*prev)
                    S.op('dve', lambda e: e.reciprocal(out=orec[:, :], in_=oacc[:, :, 128]), reads=['sb:oacc'], writes=['sb:orec'])
                    for c in range(NT):
                        S.op('dve' if c % 2 == 0 else 'pool', lambda e, c=c: e.tensor_scalar(out=oab[c % 2][:, :], in0=oacc[:, c, 0:128], scalar1=orec[:, c:c + 1], scalar2=None, op0=ALU.mult),
                             reads=['sb:oacc', 'sb:orec'], writes=['sb:oab%d' % (c % 2)])
                        tps = ps[2 + c % 2][:, 0:64].bitcast(BF16)
                        S.op('pe', lambda e, c=c, tps=tps: e.transpose(tps, oab[c % 2][:, :], ident_b), reads=['sb:oab%d' % (c % 2), 'sb:cst_b'], writes=[P[2 + c % 2]])
                        S.op('act', lambda e, c=c, tps=tps: e.activation(out=ob_[:, c * 128:(c + 1) * 128], in_=tps, func=AF.Copy),
                             reads=[P[2 + c % 2]], writes=[on_])
                    S.dma('sp', lambda e, h=h: e.dma_start(out=mixTd[h], in_=ob_[:, :]), reads=[on_], writes=['dr:mixTd'], acc=True)
                S.barrier()
            if stop_after == 'C1':
                if debug:
                    d = dbg_out("dbg_mixT", [KT, 128, S_], BF16)
                    S.dma('sp', lambda e: e.dma_start(out=d[0:8], in_=mixTd[0:8]), reads=['dr:mixTd'], writes=['dr:dbg_mixT'])
                S.finish()
                return nc, dbg

            with contextlib.ExitStack() as es:
                wG = [es.enter_context(SBT("wG%d" % i, [128, KT, 784], BF16)) for i in range(2)]
                gaA2 = [es.enter_context(SBT("gaA%d" % z, [128, 32], F32)) for z in range(2)]
                gaT2 = [es.enter_context(SBT("gaT%d" % z, [32, 128], F32)) for z in range(2)]
                lsp2 = [es.enter_context(SBT("lsp%d" % z, [128, 128], F32)) for z in range(2)]
                E12 = [es.enter_context(SBT("E1%d" % z, [128, 128], F32)) for z in range(2)]
                E22 = [es.enter_context(SBT("E2%d" % z, [128, 128], F32)) for z in range(2)]
                E32 = [es.enter_context(SBT("E3%d" % z, [128, 128], F32)) for z in range(2)]
                qdki2 = [es.enter_context(SBT("qdki%d" % z, [128, 2, 128], BF16)) for z in range(2)]
                keb2 = [es.enter_context(SBT("keb%d" % z, [128, 128], BF16)) for z in range(2)]
                qkTg2 = [es.enter_context(SBT("qkTg%d" % z, [128, 2, 128], BF16)) for z in range(2)]
                vb2 = [es.enter_context(SBT("vb%d" % z, [128, 256], BF16)) for z in range(2)]
                sg2 = [es.enter_context(SBT("sg%d" % z, [128, 256], F32)) for z in range(2)]
                attb2 = [es.enter_context(SBT("attb%d" % z, [128, 128], BF16)) for z in range(2)]
                Sf = es.enter_context(SBT("Sf", [128, 256], F32))
                Sb = [es.enter_context(SBT("Sb%d" % i, [128, 256], BF16)) for i in range(2)]
                tmpo2 = [es.enter_context(SBT("tmpo%d" % z, [128, 256], F32)) for z in range(2)]
                obb2 = [es.enter_context(SBT("obb%d" % z, [128, 256], BF16)) for z in range(2)]
                junk22 = [es.enter_context(SBT("junk2%d" % z, [128, 256], BF16)) for z in range(2)]
                oT2 = [es.enter_context(SBT("oT2_%d" % i, [128, 2, S_], BF16)) for i in range(2)]
                for z in range(2):
                    S.op('pool', lambda e, z=z: e.memset(gaA2[z][:, :], 1.0), writes=['sb:gaA%d' % z])

                NTG = DBG['gla_tiles']

                def load_wG(h):
                    buf = wG[h % 2]
                    segs = [(3072 + h * 128, 128, 0), (3584 + h * 128, 128, 128), (4096 + h * 256, 256, 256),
                            (5120 + h * 256, 256, 512), (6144, 16, 768)]
                    for j, (c0, n, o0) in enumerate(segs):
                        S.dma('pool', lambda e, buf=buf, c0=c0, n=n, o0=o0: e.dma_start(
                            out=buf[:, :, o0:o0 + n], in_=wl[:, :, c0:c0 + n]),
                            reads=['dr:w_in'], writes=['sb:wG%d' % (h % 2)], acc=(j > 0))
                load_wG(0)
                for h in range(DBG['gla_heads']):
                    if h + 1 < DBG['gla_heads']:
                        load_wG(h + 1)
                    buf = wG[h % 2]
                    wn = 'sb:wG%d' % (h % 2)
                    o2 = oT2[h % 2]
                    o2n = 'sb:oT2_%d' % (h % 2)
                    def gla_in(h, i, buf=buf, wn=wn, o2=o2, o2n=o2n):
                            gaA, gaT, lsp, E1, E2, E3, qdki, keb, qkTg, vb, sg, attb, tmpo, obb, junk2 = gaA2[i % 2], gaT2[i % 2], lsp2[i % 2], E12[i % 2], E22[i % 2], E32[i % 2], qdki2[i % 2], keb2[i % 2], qkTg2[i % 2], vb2[i % 2], sg2[i % 2], attb2[i % 2], tmpo2[i % 2], obb2[i % 2], junk22[i % 2]
                            pz = str(i % 2)
                            smc = lambda kk, n=1: sm[:, kk + 26 * (i % 2):kk + 26 * (i % 2) + n]
                            pA = [0, 2][i % 2]
                            pB = [1, 7][i % 2]
                            S.group('pe', [lambda e, k=k, i=i: e.matmul(ps[pA][:, 0:512], lhsT=hT[:, k, i * 128:(i + 1) * 128], rhs=buf[:, k, 0:512],
                                                                        start=(k == 0), stop=(k == KT - 1)) for k in range(KT)],
                                    reads=['sb:hT', wn], writes=[P[pA]])
                            S.group('pe', [lambda e, k=k, i=i: e.matmul(ps[pB][:, 0:272], lhsT=hT[:, k, i * 128:(i + 1) * 128], rhs=buf[:, k, 512:784],
                                                                        start=(k == 0), stop=(k == KT - 1)) for k in range(KT)],
                                    reads=['sb:hT', wn], writes=[P[pB]])
                    def gla_mid(h, i, buf=buf, wn=wn, o2=o2, o2n=o2n):
                            gaA, gaT, lsp, E1, E2, E3, qdki, keb, qkTg, vb, sg, attb, tmpo, obb, junk2 = gaA2[i % 2], gaT2[i % 2], lsp2[i % 2], E12[i % 2], E22[i % 2], E32[i % 2], qdki2[i % 2], keb2[i % 2], qkTg2[i % 2], vb2[i % 2], sg2[i % 2], attb2[i % 2], tmpo2[i % 2], obb2[i % 2], junk22[i % 2]
                            pz = str(i % 2)
                            smc = lambda kk, n=1: sm[:, kk + 26 * (i % 2):kk + 26 * (i % 2) + n]
                            pA = [0, 2][i % 2]
                            pB = [1, 7][i % 2]
                            S.op('act', lambda e: e.activation(out=gaA[:, 0:16], in_=ps[pB][:, 256:272], func=AF.Copy), reads=[P[pB]], writes=['sb:gaA' + pz])
                            S.op('pe', lambda e: e.transpose(ps[4][0:32, 128:256], gaA[:, 0:32], ident_f), reads=['sb:gaA' + pz, 'sb:cst_f'], writes=[P[4]])
                            S.op('act', lambda e: e.activation(out=gaT[:, :], in_=ps[4][0:32, 128:256], func=AF.Copy), reads=[P[4]], writes=['sb:gaT' + pz])
                            S.op('pe', lambda e, h=h: e.matmul(ps[3][:, 0:128], lhsT=gaT[0:17, :], rhs=wgk[0:17, l, h * 128:(h + 1) * 128], start=True, stop=True),
                                 reads=['sb:gaT' + pz, 'sb:wgk'], writes=[P[3]])
                            S.op('act', lambda e: e.activation(out=lsp[:, :], in_=ps[3][:, 0:128], func=AF.Exp, scale=-1.0), reads=[P[3]], writes=['sb:lsp' + pz])
                            S.op('act', lambda e: e.activation(out=lsp[:, :], in_=lsp[:, :], func=AF.Ln, bias=1.0), reads=['sb:lsp' + pz], writes=['sb:lsp' + pz])
                            S.op('pe', lambda e: e.matmul(ps[3][:, 128:256], lhsT=triI_f, rhs=lsp[:, :], start=True, stop=True),
                                 reads=['sb:lsp' + pz, 'sb:cst_f'], writes=[P[3]])
                            S.op('pe', lambda e: e.matmul(ps[3][:, 256:384], lhsT=triR_f, rhs=lsp[:, :], start=True, stop=True),
                                 reads=['sb:lsp' + pz, 'sb:cst_f'], writes=[P[3]])
                            S.op('pe', lambda e: e.matmul(ps[3][:, 384:385], lhsT=lsp[:, :], rhs=ones_f[:, 0:1], start=True, stop=True),
                                 reads=['sb:lsp' + pz, 'sb:cst_f'], writes=[P[3]])
                            if i + 1 < NTG:
                                gla_in(h, i + 1)
                            gs = 1.0 / 16.0
                            S.op('act', lambda e: e.activation(out=E1[:, :], in_=ps[3][:, 128:256], func=AF.Exp, scale=-gs), reads=[P[3]], writes=['sb:E1' + pz])
                            S.op('act', lambda e: e.activation(out=E2[:, :], in_=ps[3][:, 128:256], func=AF.Exp, scale=gs), reads=[P[3]], writes=['sb:E2' + pz])
                            S.op('act', lambda e: e.activation(out=E3[:, :], in_=ps[3][:, 256:384], func=AF.Exp, scale=-gs), reads=[P[3]], writes=['sb:E3' + pz])
                            S.op('act', lambda e: e.activation(out=smc(4), in_=ps[3][:, 384:385], func=AF.Exp, scale=-gs), reads=[P[3]], writes=['sb:dec' + pz])
                            S.op('dve', lambda e: e.scalar_tensor_tensor(out=qdki[:, 0, :], in0=ps[pA][:, 0:128], scalar=GK_SCALE, in1=E1[:, :], op0=ALU.mult, op1=ALU.mult),
                                 reads=[P[pA], 'sb:E1' + pz], writes=['sb:qdki' + pz])
                            S.op('dve', lambda e: e.tensor_tensor(out=qdki[:, 1, :], in0=ps[pA][:, 128:256], in1=E2[:, :], op=ALU.mult),
                                 reads=[P[pA], 'sb:E2' + pz], writes=['sb:qdki' + pz])
                            S.op('dve', lambda e: e.tensor_tensor(out=keb[:, :], in0=ps[pA][:, 128:256], in1=E3[:, :], op=ALU.mult),
                                 reads=[P[pA], 'sb:E3' + pz], writes=['sb:keb' + pz])
                            S.op('act', lambda e: e.activation(out=vb[:, :], in_=ps[pA][:, 256:512], func=AF.Copy), reads=[P[pA]], writes=['sb:vb' + pz])
                            tps = ps[4][:, 0:128].bitcast(BF16)
                            S.group('pe', [lambda e, a=a: e.transpose(tps[:, a * 128:(a + 1) * 128], qdki[:, a, :], ident_b) for a in range(2)],
                                    reads=['sb:qdki' + pz, 'sb:cst_b'], writes=[P[4]])
                            S.op('dve', lambda e: e.tensor_copy(out=qkTg[:, :, :], in_=tps.rearrange("p (a t) -> p a t", a=2)), reads=[P[4]], writes=['sb:qkTg' + pz])
                            S.op('pe', lambda e: e.matmul(ps[5][:, 0:128], lhsT=qkTg[:, 1, :], rhs=qkTg[:, 0, :], start=True, stop=True),
                                 reads=['sb:qkTg' + pz], writes=[P[5]])
                            S.op('dve', lambda e: e.tensor_tensor(out=attb[:, :], in0=ps[5][:, 0:128], in1=triI_f, op=ALU.mult),
                                 reads=[P[5], 'sb:cst_f'], writes=['sb:attb' + pz])
                            sprev = Sb[(i + 1) % 2]
                            spn = 'sb:Sb%d' % ((i + 1) % 2)
                            scur = Sb[i % 2]
                            scn = 'sb:Sb%d' % (i % 2)
                            mm = [lambda e: e.matmul(ps[6][:, 0:256], lhsT=attb[:, :], rhs=vb[:, :], start=True, stop=(i == 0))]
                            rd = ['sb:attb' + pz, 'sb:vb' + pz]
                            if i > 0:
                                mm.append(lambda e, sprev=sprev: e.matmul(ps[6][:, 0:256], lhsT=qkTg[:, 0, :], rhs=sprev[:, :], start=False, stop=True))
                                rd += ['sb:qkTg' + pz, spn]
                            S.group('pe', mm, reads=rd, writes=[P[6]])
                            S.op('pe', lambda e: e.matmul(ps[5][:, 128:384], lhsT=keb[:, :], rhs=vb[:, :], start=True, stop=True),
                                 reads=['sb:keb' + pz, 'sb:vb' + pz], writes=[P[5]])
                            if i == 0:
                                S.op('dve', lambda e: e.tensor_copy(out=Sf[:, :], in_=ps[5][:, 128:384]), reads=[P[5]], writes=['sb:Sf'])
                            else:
                                S.op('dve', lambda e: e.scalar_tensor_tensor(out=Sf[:, :], in0=Sf[:, :], scalar=smc(4), in1=ps[5][:, 128:384], op0=ALU.mult, op1=ALU.add),
                                     reads=['sb:Sf', 'sb:dec' + pz, P[5]], writes=['sb:Sf'])
                            S.op('dve', lambda e, scur=scur: e.tensor_copy(out=scur[:, :], in_=Sf[:, :]), reads=['sb:Sf'], writes=[scn])
                            S.op('act', lambda e: e.activation(out=sg[:, :], in_=ps[pB][:, 0:256], func=AF.Exp, scale=-1.0), reads=[P[pB]], writes=['sb:sg' + pz])
                            S.op('dve', lambda e: e.tensor_scalar(out=sg[:, :], in0=sg[:, :], scalar1=1.0, scalar2=None, op0=ALU.add), reads=['sb:sg' + pz], writes=['sb:sg' + pz])
                            S.op('dve', lambda e: e.reciprocal(out=sg[:, :], in_=sg[:, :]), reads=['sb:sg' + pz], writes=['sb:sg' + pz])
                            S.op('dve', lambda e: e.tensor_tensor(out=sg[:, :], in0=ps[pB][:, 0:256], in1=sg[:, :], op=ALU.mult), reads=[P[pB], 'sb:sg' + pz], writes=['sb:sg' + pz])
                            S.op('act', lambda e: e.activation(out=junk2[:, :], in_=ps[6][:, 0:256], func=AF.Square, accum_out=smc(5)),
                                 reads=[P[6]], writes=['sb:junk2' + pz, 'sb:go' + pz + '_ss'])
                            rstd_from_ss(smc(5), smc(6), 256, 'go' + pz)
                            S.op('dve', lambda e: e.scalar_tensor_tensor(out=tmpo[:, :], in0=ps[6][:, 0:256], scalar=smc(6), in1=gon[:, l, :], op0=ALU.mult, op1=ALU.mult),
                                 reads=[P[6], 'sb:go' + pz + '_rs', 'sb:gon'], writes=['sb:tmpo' + pz])
                            S.op('dve', lambda e: e.tensor_tensor(out=obb[:, :], in0=tmpo[:, :], in1=sg[:, :], op=ALU.mult),
                                 reads=['sb:tmpo' + pz, 'sb:sg' + pz], writes=['sb:obb' + pz])
                    def gla_out(h, i, buf=buf, wn=wn, o2=o2, o2n=o2n):
                            gaA, gaT, lsp, E1, E2, E3, qdki, keb, qkTg, vb, sg, attb, tmpo, obb, junk2 = gaA2[i % 2], gaT2[i % 2], lsp2[i % 2], E12[i % 2], E22[i % 2], E32[i % 2], qdki2[i % 2], keb2[i % 2], qkTg2[i % 2], vb2[i % 2], sg2[i % 2], attb2[i % 2], tmpo2[i % 2], obb2[i % 2], junk22[i % 2]
                            pz = str(i % 2)
                            smc = lambda kk, n=1: sm[:, kk + 26 * (i % 2):kk + 26 * (i % 2) + n]
                            pA = [0, 2][i % 2]
                            pB = [1, 7][i % 2]
                            tp2 = ps[4][:, 256:384].bitcast(BF16)
                            S.group('pe', [lambda e, a=a: e.transpose(tp2[:, a * 128:(a + 1) * 128], obb[:, a * 128:(a + 1) * 128], ident_b) for a in range(2)],
                                    reads=['sb:obb' + pz, 'sb:cst_b'], writes=[P[4]])
                            S.op('act', lambda e, i=i: e.activation(out=o2[:, :, i * 128:(i + 1) * 128], in_=tp2.rearrange("p (a t) -> p a t", a=2), func=AF.Copy),
                                 reads=[P[4]], writes=[o2n])
                    gla_in(h, 0)
                    for i in range(NTG):
                        gla_mid(h, i)
                        gla_out(h, i)
                    for a in range(2):
                        S.dma('sp', lambda e, a=a, h=h: e.dma_start(out=mixTd[8 + 2 * h + a], in_=o2[:, a, :]), reads=[o2n], writes=['dr:mixTd'],
                              slot='st_oT2_%d' % (h % 2), acc=True)
                S.barrier()
        if stop_after == 'C2':
            if debug:
                d = dbg_out("dbg_mixT", [KT, 128, S_], BF16)
                S.dma('sp', lambda e: e.dma_start(out=d[:, :, :], in_=mixTd[:, :, :]), reads=['dr:mixTd'], writes=['dr:dbg_mixT'])
            S.finish()
            return nc, dbg

        with contextlib.ExitStack() as esD:
            gt2b = esD.enter_context(SBT("gt2b", [128, D_], F32))
            dg = esD.enter_context(SBT("dg", [128, 128], F32))
            with contextlib.ExitStack() as es:
                mts = [es.enter_context(SBT("mt%d" % i, [128, KT, 128], BF16)) for i in range(2)]
                wo = es.enter_context(SBT("wo", [128, KT, D_], BF16))
                g2b = es.enter_context(SBT("g2b", [128, D_], F32))
                sh2b = es.enter_context(SBT("sh2b", [128, D_], F32))
                xt = [es.enter_context(SBT("xtd%d" % i, [128, D_], F32)) for i in range(2)]
                tmpx = es.enter_context(SBT("tmpx", [128, D_], F32))
                h2f2 = [es.enter_context(SBT("h2f%d" % z, [128, D_], F32)) for z in range(2)]
                h2b = [es.enter_context(SBT("h2b%d" % i, [128, D_], BF16)) for i in range(2)]
                h2T2 = [es.enter_context(SBT("h2T%d" % z, [128, KT, 128], F32)) for z in range(2)]
                junk = es.enter_context(SBT("junkd", [128, D_], BF16))
                lg2 = [es.enter_context(SBT("lg%d" % z, [128, 36], F32)) for z in range(2)]
                rt2 = [es.enter_context(SBT("rt%d" % z, [128, 8, 32], F32)) for z in range(2)]
                wov = w_out[l].rearrange("(k p) n -> p k n", p=128)
                for q4 in range(8):
                    for ch in range(2):
                        S.dma('pool', lambda e, q4=q4, ch=ch: e.dma_start(out=wo[:, q4 * 2:(q4 + 1) * 2, ch * 1024:(ch + 1) * 1024],
                                                                         in_=wov[:, q4 * 2:(q4 + 1) * 2, ch * 1024:(ch + 1) * 1024]),
                              reads=['dr:w_out'], writes=['sb:wo'], acc=(q4 + ch > 0))
                bcast(tmpx, gt1, dg, 'sb:tmpx0', ['sb:modT'])
                for k in range(KT):
                    S.op('dve' if k % 2 == 0 else 'pool', lambda e, k=k: e.tensor_tensor(out=wo[:, k, :], in0=wo[:, k, :], in1=tmpx[:, :], op=ALU.mult),
                         reads=['sb:wo', 'sb:tmpx0'], writes=['sb:wo'])
                bcast(gt2b, gt2, dg, 'sb:gt2b', ['sb:modT'])
                bcast(g2b, lambda kt: g2T[:, kt:kt + 1], dg, 'sb:g2b', ['sb:g2T'])
                bcast(sh2b, sh2, dg, 'sb:sh2b', ['sb:modT'])
                S.op('pool', lambda e: e.memset(selacc[:, :], 0.0), writes=['sb:selacc'])
                Xgv = Xg.rearrange("(n p) d -> p n d", p=128)
                def d_big(i):
                        xb = xt[i % 2]
                        xn_ = 'sb:xtd%d' % (i % 2)
                        h2f, h2T, lg, rt = h2f2[i % 2], h2T2[i % 2], lg2[i % 2], rt2[i % 2]
                        pz = str(i % 2)
                        smd = lambda kk, n=1, i=i: sm[:, kk + 26 * (i % 2):kk + 26 * (i % 2) + n]
                        hb = h2b[i % 2]
                        hbn = 'sb:h2b%d' % (i % 2)
                        S.dma('sp', lambda e, xb=xb, i=i: e.dma_start(out=xb[:, :], in_=x_src[i * 128:(i + 1) * 128, :]), reads=[xres], writes=[xn_])
                        mt = mts[i % 2]
                        mtn = 'sb:mt%d' % (i % 2)
                        S.dma('sp', lambda e, mt=mt, i=i: e.dma_start(out=mt[:, :, :], in_=mixTd[:, :, i * 128:(i + 1) * 128].rearrange("k p s -> p k s")),
                              reads=['dr:mixTd'], writes=[mtn])
                        for n4 in range(4):
                            S.group('pe', [lambda e, k=k, mt=mt, n4=n4: e.matmul(ps[n4][:, :], lhsT=mt[:, k, :], rhs=wo[:, k, n4 * 512:(n4 + 1) * 512],
                                                                               start=(k == 0), stop=(k == KT - 1)) for k in range(KT)],
                                    reads=[mtn, 'sb:wo'], writes=[P[n4]])

                def d_post(i):
                        xb = xt[i % 2]
                        xn_ = 'sb:xtd%d' % (i % 2)
                        h2f, h2T, lg, rt = h2f2[i % 2], h2T2[i % 2], lg2[i % 2], rt2[i % 2]
                        pz = str(i % 2)
                        smd = lambda kk, n=1, i=i: sm[:, kk + 26 * (i % 2):kk + 26 * (i % 2) + n]
                        hb = h2b[i % 2]
                        hbn = 'sb:h2b%d' % (i % 2)
                        for n4 in range(4):
                            S.op('dve', lambda e, n4=n4, xb=xb: e.tensor_tensor(out=xb[:, n4 * 512:(n4 + 1) * 512], in0=ps[n4][:, :], in1=xb[:, n4 * 512:(n4 + 1) * 512], op=ALU.add),
                                 reads=[P[n4], xn_], writes=[xn_])
                        S.dma('sp', lambda e, xb=xb, i=i: e.dma_start(out=xd[i * 128:(i + 1) * 128, :], in_=xb[:, :]), reads=[xn_], writes=['dr:xd_w'], acc=True)
                        S.op('act', lambda e, xb=xb: e.activation(out=junk[:, :], in_=xb[:, :], func=AF.Square, accum_out=smd(8)),
                             reads=[xn_], writes=['sb:junkd', 'sb:n2' + pz + '_ss'])
                        rstd_from_ss(smd(8), smd(9), D_, 'n2' + pz)
                        S.op('dve', lambda e, xb=xb: e.scalar_tensor_tensor(out=tmpx[:, :], in0=xb[:, :], scalar=smd(9), in1=g2b[:, :], op0=ALU.mult, op1=ALU.mult),
                             reads=[xn_, 'sb:n2' + pz + '_rs', 'sb:g2b'], writes=['sb:tmpx0', 'sb:tmpx1', 'sb:tmpx2', 'sb:tmpx3'])
                        S.op('dve', lambda e: e.tensor_tensor(out=h2f[:, :], in0=tmpx[:, :], in1=sh2b[:, :], op=ALU.add),
                             reads=['sb:tmpx0', 'sb:tmpx1', 'sb:tmpx2', 'sb:tmpx3', 'sb:sh2b'], writes=['sb:h2f' + pz])
                        hb = h2b[i % 2]
                        hbn = 'sb:h2b%d' % (i % 2)
                        S.op('act', lambda e, hb=hb: e.activation(out=hb[:, :], in_=h2f[:, :], func=AF.Copy), reads=['sb:h2f' + pz], writes=[hbn])

                def d_rA(i):
                        xb = xt[i % 2]
                        xn_ = 'sb:xtd%d' % (i % 2)
                        h2f, h2T, lg, rt = h2f2[i % 2], h2T2[i % 2], lg2[i % 2], rt2[i % 2]
                        pz = str(i % 2)
                        smd = lambda kk, n=1, i=i: sm[:, kk + 26 * (i % 2):kk + 26 * (i % 2) + n]
                        hb = h2b[i % 2]
                        hbn = 'sb:h2b%d' % (i % 2)
                        for g4 in range(4):
                            bank = 4 + g4 % 2
                            S.group('pe', [lambda e, kt=kt, bank=bank: e.transpose(ps[bank][:, (kt % 4) * 128:(kt % 4 + 1) * 128], h2f[:, kt * 128:(kt + 1) * 128], ident_f)
                                           for kt in range(g4 * 4, g4 * 4 + 4)], reads=['sb:h2f' + pz, 'sb:cst_f'], writes=[P[bank]])
                            S.op('act', lambda e, g4=g4, bank=bank: e.activation(out=h2T[:, g4 * 4:(g4 + 1) * 4, :], in_=ps[bank][:, :].rearrange("p (a t) -> p a t", a=4), func=AF.Copy),
                                 reads=[P[bank]], writes=['sb:h2T' + pz])
                        mm = [lambda e, k=k: e.matmul(ps[6][:, 0:36], lhsT=h2T[:, k, :], rhs=wr[:, l, k, :], start=(k == 0), stop=False) for k in range(KT)]
                        mm.append(lambda e: e.matmul(ps[6][:, 0:36], lhsT=ones_f[0:1, :], rhs=br[0:1, l, :], start=False, stop=True))
                        S.group('pe', mm, reads=['sb:h2T' + pz, 'sb:wr', 'sb:br', 'sb:cst_f'], writes=[P[6]])
                def d_rB(i):
                        xb = xt[i % 2]
                        xn_ = 'sb:xtd%d' % (i % 2)
                        h2f, h2T, lg, rt = h2f2[i % 2], h2T2[i % 2], lg2[i % 2], rt2[i % 2]
                        pz = str(i % 2)
                        smd = lambda kk, n=1, i=i: sm[:, kk + 26 * (i % 2):kk + 26 * (i % 2) + n]
                        hb = h2b[i % 2]
                        hbn = 'sb:h2b%d' % (i % 2)
                        R_ = ['sb:rt' + pz]
                        S.op('dve', lambda e: e.tensor_copy(out=lg[:, :], in_=ps[6][:, 0:36]), reads=[P[6]], writes=['sb:lg' + pz])
                        S.op('dve', lambda e: e.tensor_reduce(out=smd(10), in_=lg[:, 0:4], axis=AX.X, op=ALU.max), reads=['sb:lg' + pz], writes=R_)
                        S.op('dve', lambda e: e.tensor_scalar(out=rt[:, 0, 0:4], in0=lg[:, 0:4], scalar1=smd(10), scalar2=None, op0=ALU.is_ge), reads=['sb:lg' + pz] + R_, writes=R_)
                        S.op('dve', lambda e: e.tensor_scalar(out=smd(11), in0=smd(10), scalar1=-1.0, scalar2=None, op0=ALU.mult), reads=R_, writes=R_)
                        S.op('act', lambda e: e.activation(out=rt[:, 0, 8:12], in_=lg[:, 0:4], func=AF.Exp, bias=smd(11), accum_out=smd(12)), reads=['sb:lg' + pz] + R_, writes=R_)
                        S.op('dve', lambda e: e.tensor_scalar(out=rt[:, 0, 4:8], in0=rt[:, 0, 0:4], scalar1=-NEG, scalar2=NEG, op0=ALU.mult, op1=ALU.add), reads=R_, writes=R_)
                        S.op('dve', lambda e: e.tensor_tensor(out=rt[:, 1, :].rearrange("p (g j) -> p g j", g=4), in0=lg[:, 4:36].rearrange("p (g j) -> p g j", g=4),
                                                              in1=rt[:, 0, 4:8].unsqueeze(2).to_broadcast([128, 4, 8]), op=ALU.add), reads=['sb:lg' + pz] + R_, writes=R_)
                        S.op('dve', lambda e: e.max(out=rt[:, 0, 16:24], in_=rt[:, 1, :]), reads=R_, writes=R_)
                        v0 = rt[:, 0, 16:17]
                        v1 = rt[:, 0, 17:18]
                        S.op('dve', lambda e: e.tensor_scalar(out=rt[:, 2, :], in0=rt[:, 1, :], scalar1=v0, scalar2=None, op0=ALU.is_equal), reads=R_, writes=R_)
                        S.op('dve', lambda e: e.tensor_scalar(out=rt[:, 3, :], in0=rt[:, 1, :], scalar1=v1, scalar2=None, op0=ALU.is_equal), reads=R_, writes=R_)
                        S.op('dve', lambda e: e.tensor_tensor(out=smd(13), in0=v1, in1=v0, op=ALU.subtract), reads=R_, writes=R_)
                        S.op('act', lambda e: e.activation(out=smd(14), in_=smd(13), func=AF.Exp), reads=R_, writes=R_)
                        S.op('dve', lambda e: e.scalar_tensor_tensor(out=smd(15), in0=smd(14), scalar=1.0, in1=smd(12), op0=ALU.add, op1=ALU.mult), reads=R_, writes=R_)
                        S.op('dve', lambda e: e.reciprocal(out=wts[:, i, 0:1], in_=smd(15)), reads=R_, writes=['sb:wts'])
                        S.op('dve', lambda e: e.tensor_tensor(out=wts[:, i, 1:2], in0=wts[:, i, 0:1], in1=smd(14), op=ALU.mult), reads=['sb:wts'] + R_, writes=['sb:wts'])
                        S.op('dve', lambda e: e.tensor_tensor(out=rt[:, 4, :], in0=rt[:, 2, :], in1=rt[:, 3, :], op=ALU.add), reads=R_, writes=R_)
                        S.group('pe', [lambda e: e.matmul(ps[7][:, 0:32], lhsT=triS_f, rhs=rt[:, 4, :], start=True, stop=False),
                                       lambda e: e.matmul(ps[7][:, 0:32], lhsT=ones_f, rhs=selacc[:, :], start=False, stop=True)],
                                reads=R_ + ['sb:selacc', 'sb:cst_f'], writes=[P[7]])
                        S.op('dve', lambda e: e.tensor_tensor(out=selacc[:, :], in0=selacc[:, :], in1=rt[:, 4, :], op=ALU.add), reads=R_ + ['sb:selacc'], writes=['sb:selacc'])
                        S.op('dve', lambda e: e.tensor_scalar(out=rt[:, 5, :], in0=ps[7][:, 0:32], scalar1=float(CAP), scalar2=1.0e6, op0=ALU.is_ge, op1=ALU.mult), reads=[P[7]], writes=R_)
                        S.op('dve', lambda e: e.tensor_tensor(out=rt[:, 6, :], in0=ps[7][:, 0:32], in1=ebase[:, :], op=ALU.add), reads=[P[7], 'sb:ebase'], writes=R_)
                        S.op('dve', lambda e: e.tensor_tensor(out=rt[:, 6, :], in0=rt[:, 6, :], in1=rt[:, 5, :], op=ALU.add), reads=R_, writes=R_)
                        for a, idx in ((2, idx_a), (3, idx_b)):
                            S.op('dve', lambda e, a=a: e.tensor_tensor(out=rt[:, 7, :], in0=rt[:, a, :], in1=rt[:, 6, :], op=ALU.mult), reads=R_, writes=R_)
                            S.op('dve', lambda e: e.tensor_reduce(out=smd(16), in_=rt[:, 7, :], axis=AX.X, op=ALU.add), reads=R_, writes=R_)
                            S.op('dve', lambda e, idx=idx, i=i: e.tensor_copy(out=idx[:, i:i + 1], in_=smd(16)), reads=R_, writes=['sb:idx'])
                        for idx in (idx_a, idx_b):
                            S.dma('pool', lambda e, idx=idx, hb=hb, i=i: e.indirect_dma_start(
                                out=Xg[:, :], out_offset=bass.IndirectOffsetOnAxis(ap=idx[:, i:i + 1], axis=0),
                                in_=hb[:, :], in_offset=None, bounds_check=bc_reg, oob_is_err=False),
                                reads=[hbn, 'sb:idx'], writes=['dr:Xg'], slot='st_h2b%d' % (i % 2), acc=True)
                d_big(0)
                d_post(0)
                d_big(1)
                d_rA(0)
                for i in range(NT):
                    if i + 1 < NT:
                        d_post(i + 1)
                    if i + 2 < NT:
                        d_big(i + 2)
                    d_rB(i)
                    if i + 1 < NT:
                        d_rA(i + 1)
                S.barrier()
            if stop_after == 'D':
                if debug:
                    d = dbg_out("dbg_idx", [128, 2, NT], I32)
                    S.dma('sp', lambda e: e.dma_start(out=d[:, 0, :], in_=idx_a[:, :]), reads=['sb:idx'], writes=['dr:dbg_idx'])
                    S.dma('sp', lambda e: e.dma_start(out=d[:, 1, :], in_=idx_b[:, :]), reads=['sb:idx'], writes=['dr:dbg_idx2'])
                    d2 = dbg_out("dbg_wts", [128, NT, 2])
                    S.dma('sp', lambda e: e.dma_start(out=d2[:, :, :], in_=wts[:, :, :]), reads=['sb:wts'], writes=['dr:dbg_wts'])
                    d3 = dbg_out("dbg_x1", [S_, D_])
                    S.dma('sp', lambda e: e.dma_start(out=d3[:, :], in_=xd[:, :]), reads=['dr:xd_w'], writes=['dr:dbg_x1'])
                S.finish()
                return nc, dbg

            NST = CAP // 128
            with contextlib.ExitStack() as es:
                ring = [es.enter_context(SBT("wr%d" % i, [128, 16 * 512], BF16)) for i in range(4)]
                xe = [es.enter_context(SBT("xe%d" % i, [128, NST, D_], BF16)) for i in range(2)]
                xeT = es.enter_context(SBT("xeT", [128, KT, CAP], BF16))
                sgu = es.enter_context(SBT("sgu", [128, CAP], F32))
                hTe = [es.enter_context(SBT("hTe%d" % i, [128, 8, CAP], BF16)) for i in range(2)]
                yb_ = [es.enter_context(SBT("yb%d" % i, [128, 1024], F32)) for i in range(2)]
                pieces = []
                for e_ in range(NEXP):
                    for ms in range(2):
                        pieces.append((e_, 'g', ms))
                        pieces.append((e_, 'u', ms))
                    for ns in range(2):
                        pieces.append((e_, 'd', ns))

                def load_piece(pi):
                    e_, kind, hf = pieces[pi]
                    buf = ring[pi % 4]
                    rn = 'sb:wr%d' % (pi % 4)
                    if kind in 'gu':
                        src = (w_eg if kind == 'g' else w_eu)[l, e_].rearrange("(k p) n -> p k n", p=128)[:, :, hf * 512:(hf + 1) * 512]
                        bv = buf[:, :].rearrange("p (k n) -> p k n", k=16)
                        for q in range(2):
                            S.dma('pool', lambda e, bv=bv, src=src, q=q: e.dma_start(out=bv[:, q * 8:(q + 1) * 8, :], in_=src[:, q * 8:(q + 1) * 8, :]),
                                  reads=['dr:w_e'], writes=[rn], acc=(q > 0))
                    else:
                        src = w_ed[l, e_].rearrange("(k p) n -> p k n", p=128)[:, :, hf * 1024:(hf + 1) * 1024]
                        bv = buf[:, :].rearrange("p (k n) -> p k n", k=8)
                        for q in range(2):
                            S.dma('pool', lambda e, bv=bv, src=src, q=q: e.dma_start(out=bv[:, q * 4:(q + 1) * 4, :], in_=src[:, q * 4:(q + 1) * 4, :]),
                                  reads=['dr:w_e'], writes=[rn], acc=(q > 0))
                LOOK = 3
                for pi in range(LOOK):
                    load_piece(pi)

                def load_xe(e_):
                    S.dma('sp', lambda e, e_=e_: e.dma_start(out=xe[e_ % 2][:, :, :], in_=Xg[e_ * CAP:(e_ + 1) * CAP, :].rearrange("(s p) d -> p s d", p=128)),
                          reads=['dr:Xg'], writes=['sb:xe%d' % (e_ % 2)])
                load_xe(0)
                pi = 0
                ycnt = 0
                for e_ in range(NEXP):
                    if e_ + 1 < NEXP:
                        load_xe(e_ + 1)
                    xb = xe[e_ % 2]
                    xbn = 'sb:xe%d' % (e_ % 2)
                    xT = xeT
                    xTn = 'sb:xeT'
                    hE = hTe[e_ % 2]
                    hEn = 'sb:hTe%d' % (e_ % 2)
                    KPB = 1024 // CAP
                    for g in range(KT // KPB):
                        bank = g % 2
                        tp = ps[bank][:, :].bitcast(BF16)
                        S.group('pe', [lambda e, kt=kt, st=st, tp=tp: e.transpose(tp[:, (kt % KPB) * CAP + st * 128:(kt % KPB) * CAP + (st + 1) * 128],
                                                                                 xb[:, st, kt * 128:(kt + 1) * 128], ident_b)
                                       for kt in range(g * KPB, (g + 1) * KPB) for st in range(NST)], reads=[xbn, 'sb:cst_b'], writes=[P[bank]])
                        if g % 2 == 0:
                            S.op('dve', lambda e, g=g, tp=tp: e.tensor_copy(out=xT[:, g * KPB:(g + 1) * KPB, :], in_=tp.rearrange("p (k s) -> p k s", k=KPB)),
                                 reads=[P[bank]], writes=[xTn])
                        else:
                            S.op('act', lambda e, g=g, tp=tp: e.activation(out=xT[:, g * KPB:(g + 1) * KPB, :], in_=tp.rearrange("p (k s) -> p k s", k=KPB), func=AF.Copy),
                                 reads=[P[bank]], writes=[xTn])
                    gu = 0
                    for ms in range(2):
                        bg = ring[pi % 4][:, :].rearrange("p (k n) -> p k n", k=16)
                        bgn = 'sb:wr%d' % (pi % 4)
                        bu = ring[(pi + 1) % 4][:, :].rearrange("p (k n) -> p k n", k=16)
                        bun = 'sb:wr%d' % ((pi + 1) % 4)
                        for m in range(4):
                            bkg = 2 + 2 * (gu % 2)
                            bku = bkg + 1
                            gu += 1
                            S.group('pe', [lambda e, k=k, m=m, bkg=bkg, bg=bg: e.matmul(ps[bkg][:, 0:CAP], lhsT=bg[:, k, m * 128:(m + 1) * 128], rhs=xT[:, k, :],
                                                                                       start=(k == 0), stop=(k == KT - 1)) for k in range(KT)],
                                    reads=[bgn, xTn], writes=[P[bkg]])
                            S.group('pe', [lambda e, k=k, m=m, bku=bku, bu=bu: e.matmul(ps[bku][:, 0:CAP], lhsT=bu[:, k, m * 128:(m + 1) * 128], rhs=xT[:, k, :],
                                                                                       start=(k == 0), stop=(k == KT - 1)) for k in range(KT)],
                                    reads=[bun, xTn], writes=[P[bku]])
                            S.op('act', lambda e, bkg=bkg: e.activation(out=sgu[:, :], in_=ps[bkg][:, 0:CAP], func=AF.Silu), reads=[P[bkg]], writes=['sb:sgu'])
                            S.op('dve', lambda e, bku=bku, ms=ms, m=m: e.tensor_tensor(out=hE[:, ms * 4 + m, :], in0=ps[bku][:, 0:CAP], in1=sgu[:, :], op=ALU.mult),
                                 reads=[P[bku], 'sb:sgu'], writes=[hEn])
                        pi += 2
                        for q in range(2):
                            if pi - 2 + q + LOOK < len(pieces):
                                load_piece(pi - 2 + q + LOOK)
                    for ns in range(2):
                        bd = ring[pi % 4][:, :].rearrange("p (k n) -> p k n", k=8)
                        bdn = 'sb:wr%d' % (pi % 4)
                        for st in range(NST):
                            yb = yb_[ycnt % 2]
                            ybn = 'sb:yb%d' % (ycnt % 2)
                            ycnt += 1
                            for nn in range(2):
                                bank = 6 + nn
                                S.group('pe', [lambda e, k=k, st=st, nn=nn, bank=bank, bd=bd: e.matmul(ps[bank][:, :], lhsT=hE[:, k, st * 128:(st + 1) * 128], rhs=bd[:, k, nn * 512:(nn + 1) * 512],
                                                                                                 start=(k == 0), stop=(k == 7)) for k in range(8)],
                                        reads=[bdn, hEn], writes=[P[bank]])
                                if nn == 0:
                                    S.op('act', lambda e, bank=bank, yb=yb: e.activation(out=yb[:, 0:512], in_=ps[bank][:, :], func=AF.Copy), reads=[P[bank]], writes=[ybn])
                                else:
                                    S.op('dve', lambda e, bank=bank, yb=yb: e.tensor_copy(out=yb[:, 512:1024], in_=ps[bank][:, :]), reads=[P[bank]], writes=[ybn])
                            S.dma('sp', lambda e, st=st, e_=e_, ns=ns, yb=yb: e.dma_start(
                                out=Yg[e_ * CAP + st * 128:e_ * CAP + (st + 1) * 128, ns * 1024:(ns + 1) * 1024], in_=yb[:, :]),
                                reads=[ybn], writes=['dr:Yg'], acc=True)
                        pi += 1
                        if pi - 1 + LOOK < len(pieces):
                            load_piece(pi - 1 + LOOK)
                S.barrier()

            with contextlib.ExitStack() as es:
                ya2 = [es.enter_context(SBT("ya%d" % z, [128, D_], F32)) for z in range(2)]
                yb22 = [es.enter_context(SBT("yb2%d" % z, [128, D_], F32)) for z in range(2)]
                xt = [es.enter_context(SBT("xtf%d" % i, [128, D_], F32)) for i in range(2)]
                junk = es.enter_context(SBT("junkf", [128, D_], BF16))
                if last:
                    lnfb = es.enter_context(SBT("lnfb", [128, D_], F32))
                    bcast(lnfb, lambda kt: lnT[:, 2 * DEPTH, kt:kt + 1], dg, 'sb:lnfb', ['sb:lnT'])
                for i in range(NT):
                    ya, yb2, pz = ya2[i % 2], yb22[i % 2], str(i % 2)
                    xb = xt[i % 2]
                    xn_ = 'sb:xtf%d' % (i % 2)
                    S.dma('sp', lambda e, xb=xb, i=i: e.dma_start(out=xb[:, :], in_=xd[i * 128:(i + 1) * 128, :]), reads=['dr:xd_w'], writes=[xn_])
                    if i < 2:
                        S.op('dve', lambda e: e.memset(ya[:, :], 0.0), writes=['sb:ya' + pz])
                        S.op('dve', lambda e: e.memset(yb2[:, :], 0.0), writes=['sb:yb2' + pz])
                    for idx, yt, yn in ((idx_a, ya, 'sb:ya' + pz), (idx_b, yb2, 'sb:yb2' + pz)):
                        S.dma('pool', lambda e, idx=idx, yt=yt, i=i: e.indirect_dma_start(
                            out=yt[:, :], out_offset=None, in_=Yg[:, :], in_offset=bass.IndirectOffsetOnAxis(ap=idx[:, i:i + 1], axis=0),
                            bounds_check=bc_reg, oob_is_err=False), reads=['dr:Yg', 'sb:idx'], writes=[yn])
                    S.op('act', lambda e, i=i: e.activation(out=yb2[:, :], in_=yb2[:, :], func=AF.Copy, scale=wts[:, i, 1:2]),
                         reads=['sb:yb2' + pz, 'sb:wts'], writes=['sb:yb2' + pz])
                    S.op('dve', lambda e, i=i: e.scalar_tensor_tensor(out=ya[:, :], in0=ya[:, :], scalar=wts[:, i, 0:1], in1=yb2[:, :], op0=ALU.mult, op1=ALU.add),
                         reads=['sb:ya' + pz, 'sb:yb2' + pz, 'sb:wts'], writes=['sb:ya' + pz])
                    S.op('dve', lambda e: e.tensor_tensor(out=ya[:, :], in0=ya[:, :], in1=gt2b[:, :], op=ALU.mult), reads=['sb:ya' + pz, 'sb:gt2b'], writes=['sb:ya' + pz])
                    S.op('dve', lambda e, xb=xb: e.tensor_tensor(out=xb[:, :], in0=xb[:, :], in1=ya[:, :], op=ALU.add), reads=['sb:ya' + pz, xn_], writes=[xn_])
                    if not last:
                        S.dma('sp', lambda e, xb=xb, i=i: e.dma_start(out=xd[i * 128:(i + 1) * 128, :], in_=xb[:, :]), reads=[xn_], writes=['dr:xd'], acc=True)
                    else:
                        S.op('act', lambda e, xb=xb: e.activation(out=junk[:, :], in_=xb[:, :], func=AF.Square, accum_out=smc(20)),
                             reads=[xn_], writes=['sb:junkf', 'sb:nf_ss'])
                        rstd_from_ss(smc(20), smc(21), D_, 'nf')
                        S.op('dve', lambda e, xb=xb: e.scalar_tensor_tensor(out=xb[:, :], in0=xb[:, :], scalar=smc(21), in1=lnfb[:, :], op0=ALU.mult, op1=ALU.mult),
                             reads=[xn_, 'sb:nf_rs', 'sb:lnfb'], writes=[xn_])
                        S.dma('sp', lambda e, xb=xb, i=i: e.dma_start(out=y_out[i * 128:(i + 1) * 128, :], in_=xb[:, :]), reads=[xn_], writes=['dr:y'], acc=True)
                S.barrier()
    S.finish()
    return nc, dbg


def _consts():
    p = np.arange(128)[:, None]
    f = np.arange(128)[None, :]
    c = np.zeros((128, 6, 128), np.float32)
    c[:, 0] = (p == f)
    c[:, 1] = (p <= f)
    c[:, 2] = (p > f)
    c[:, 3] = (p < f)
    c[:, 4] = 1.0
    half = 16
    inv = np.power(np.float32(500000.0), -np.arange(half, dtype=np.float32) * np.float32(2.0 / 32)).astype(np.float32)
    ang = (np.arange(S_, dtype=np.float32)[:, None] * inv[None, :]).astype(np.float32)
    cos = np.cos(ang).astype(np.float32)
    sin = np.sin(ang).astype(np.float32)
    cs = np.concatenate([cos, cos], axis=1)
    sn = np.concatenate([-sin, sin], axis=1)
    rope = np.zeros((S_, 2, 2, 32), np.float32)
    rope[:, 0, 0] = cs * np.float32(ATTN_SCALE)
    rope[:, 0, 1] = cs
    rope[:, 1, 0] = sn * np.float32(ATTN_SCALE)
    rope[:, 1, 1] = sn
    rope = np.ascontiguousarray(rope.reshape(NT, 128, 2, 2, 32).transpose(1, 0, 2, 3, 4))
    negm = np.zeros((128, 8, 8), np.float32)
    for blk in range(8):
        negm[:, blk, blk:] = NEG
    ebase = np.broadcast_to((np.arange(NEXP, dtype=np.float32) * CAP)[None, :], (128, NEXP)).copy()
    return c, rope, negm, ebase


def _fm(v):
    v = np.asarray(v, np.float32)
    lead = v.shape[:-1]
    return np.ascontiguousarray(np.moveaxis(v.reshape(*lead, -1, 128), -1, 0))


_NC_CACHE = {}


def make_inputs(x, c, ln1, ln2, w_ada, b_ada, w_in, w_gk, b_gk, g_onorm, w_out, w_r1, b_r1, w_r2, b_r2,
                w_e_gate, w_e_up, w_e_down, ln_f):
    f = lambda a: np.ascontiguousarray(np.asarray(a, dtype=np.float32))
    x = f(x)
    consts, rope, negm, ebase = _consts()
    lnT = np.stack([_fm(ln1[0]), _fm(ln2[0]), _fm(ln1[1]), _fm(ln2[1]), _fm(ln_f)], axis=1)
    badaT = _fm(np.asarray(b_ada, np.float32))
    wgk_aug = np.ascontiguousarray(np.concatenate([np.asarray(w_gk, np.float32), np.asarray(b_gk, np.float32)[:, None, :]], axis=1).transpose(1, 0, 2))
    gon_b = np.ascontiguousarray(np.broadcast_to(np.asarray(g_onorm, np.float32)[None], (128, DEPTH, 256)))
    wrc = np.concatenate([np.asarray(w_r1, np.float32), np.asarray(w_r2, np.float32)], axis=2)
    wr = np.ascontiguousarray(wrc.reshape(DEPTH, KT, 128, 36).transpose(2, 0, 1, 3))
    br = np.ascontiguousarray(np.concatenate([np.asarray(b_r1, np.float32), np.asarray(b_r2, np.float32)], axis=1)[None])
    shared = dict(lnT=np.ascontiguousarray(lnT), badaT=badaT, w_ada=f(w_ada), w_in=f(w_in), wgk_aug=wgk_aug, gon_b=gon_b,
                  w_out=f(w_out), wr=wr, br=br, w_e_gate=f(w_e_gate), w_e_up=f(w_e_up), w_e_down=f(w_e_down),
                  rope=rope, consts=consts, negmask=negm, ebase=ebase)
    in_maps = []
    for b in range(NCORES):
        m = dict(shared)
        m["x"] = x[b]
        m["cT"] = _fm(np.asarray(c, np.float32)[b])
        in_maps.append(m)
    return in_maps


def kernel(**inputs):
    in_maps = make_inputs(**inputs)
    if 'nc' not in _NC_CACHE:
        _NC_CACHE['nc'] = build_nc()[0]
    res = run_bass_kernel_spmd(_NC_CACHE['nc'], in_maps, core_ids=list(range(NCORES)))
    return np.stack([np.asarray(r["y"], dtype=np.float32) for r in res.results], axis=0)
```

```python
import numpy as np
import concourse.bass as bass
import concourse.mybir as mybir
from concourse.bass_utils import run_bass_kernel_spmd

F32 = mybir.dt.float32
BF16 = mybir.dt.bfloat16
I32 = mybir.dt.int32
AF = mybir.ActivationFunctionType
ALU = mybir.AluOpType
AX = mybir.AxisListType

NCORES = 8
S_ = 2048
D_ = 2048
NT = 16
KT = 16
DEPTH = 2
INW = 6160
CAP = 512
NEXP = 32
NSLOT = NEXP * CAP
EPS = 1e-6
ATTN_SCALE = 128 ** -0.5
GK_SCALE = 128 ** -0.5
NEG = -1.0e30
DBG = {'skip_c1': False, 'gla_steps': 99, 'gla_tiles': NT, 'gla_heads': 4}


class Sched:
    def __init__(self, nc):
        self.nc = nc
        self.engs = {'pe': nc.tensor, 'act': nc.scalar, 'dve': nc.vector, 'pool': nc.gpsimd, 'sp': nc.sync}
        self.esem = {e: nc.alloc_semaphore('c_' + e) for e in ['pe', 'act', 'dve', 'pool']}
        self.ecnt = {e: 0 for e in self.esem}
        self.known = {e: {} for e in self.engs}
        self.res = {}
        self.dsems = {}
        self.nwait = 0

    def _wait(self, eng, ev):
        key, sem, val = ev
        if self.known[eng].get(key, 0) >= val:
            return
        self.engs[eng].wait_ge(sem, val)
        self.known[eng][key] = val
        self.nwait += 1

    def _deps(self, eng, reads, writes):
        best = {}

        def add(ev):
            if ev[0] not in best or best[ev[0]][2] < ev[2]:
                best[ev[0]] = ev
        for r in reads:
            st = self.res.get(r)
            if st:
                for ev in st['w'].values():
                    if not (ev[0] == eng and eng == 'pe'):
                        add(ev)
                if r.startswith('ps:'):
                    for ev in st['r'].values():
                        if ev[0] != eng:
                            add(ev)
        for w in writes:
            st = self.res.get(w)
            if st:
                for ev in st['w'].values():
                    if ev[0] != eng:
                        add(ev)
                for ev in st['r'].values():
                    if ev[0] != eng:
                        add(ev)
        for ev in best.values():
            self._wait(eng, ev)

    def _record(self, ev, reads, writes, acc=False):
        for r in reads:
            st = self.res.setdefault(r, {'w': {}, 'r': {}})
            st['r'][ev[0]] = ev
        for w in writes:
            if acc and w in self.res:
                self.res[w]['w'][ev[0]] = ev
                self.res[w]['r'] = {}
            else:
                self.res[w] = {'w': {ev[0]: ev}, 'r': {}}

    def op(self, eng, fn, reads=(), writes=()):
        self._deps(eng, reads, writes)
        ins = fn(self.engs[eng])
        self.ecnt[eng] += 1
        ins.then_inc(self.esem[eng], 1)
        self._record((eng, self.esem[eng], self.ecnt[eng]), reads, writes)

    def group(self, eng, fns, reads=(), writes=()):
        self._deps(eng, reads, writes)
        ins = None
        for fn in fns:
            ins = fn(self.engs[eng])
        self.ecnt[eng] += 1
        ins.then_inc(self.esem[eng], 1)
        self._record((eng, self.esem[eng], self.ecnt[eng]), reads, writes)

    def dma(self, eng, fn, reads=(), writes=(), slot=None, acc=False):
        self._deps(eng, reads, writes)
        if slot is None:
            slot = writes[0] if writes[0].startswith('sb:') else 'st_' + reads[0]
        if slot not in self.dsems:
            self.dsems[slot] = [self.nc.alloc_semaphore('d%d' % len(self.dsems)), 0]
        sc = self.dsems[slot]
        sc[1] += 16
        fn(self.engs[eng]).then_inc(sc[0], 16)
        self._record(('d_' + slot, sc[0], sc[1]), reads, writes, acc=acc)

    def barrier(self):
        evs = [(e, self.esem[e], self.ecnt[e]) for e in self.esem if self.ecnt[e] > 0]
        evs += [('d_' + s, sc[0], sc[1]) for s, sc in self.dsems.items()]
        for eng in self.engs:
            for ev in evs:
                if ev[0] != eng:
                    self._wait(eng, ev)
        self.res = {}

    def finish(self):
        evs = [(e, self.esem[e], self.ecnt[e]) for e in self.esem if self.ecnt[e] > 0]
        evs += [('d_' + s, sc[0], sc[1]) for s, sc in self.dsems.items()]
        for ev in evs:
            self._wait('sp', ev)


def build_nc(n_layers=DEPTH, stop_after=None, debug=False):
    nc = bass.Bass("TRN2", target_bir_lowering=False)
    S = Sched(nc)

    def din(name, shape, dt=F32):
        return nc.dram_tensor(name, list(shape), dt, kind="ExternalInput").ap()

    x_in = din("x", [S_, D_])
    cT_in = din("cT", [128, KT])
    lnT_in = din("lnT", [128, 2 * DEPTH + 1, KT])
    badaT_in = din("badaT", [128, DEPTH, 96])
    w_ada = din("w_ada", [DEPTH, D_, 6 * D_])
    w_in = din("w_in", [DEPTH, D_, INW])
    wgk_in = din("wgk_aug", [17, DEPTH, 512])
    gon_in = din("gon_b", [128, DEPTH, 256])
    early = ('mod', 'B', 'C1', 'C2')
    w_out = din("w_out", [DEPTH, D_, D_]) if stop_after not in early else None
    wr_in = din("wr", [128, DEPTH, KT, 36])
    br_in = din("br", [1, DEPTH, 36])
    if stop_after not in early + ('D',):
        w_eg = din("w_e_gate", [DEPTH, NEXP, D_, 1024])
        w_eu = din("w_e_up", [DEPTH, NEXP, D_, 1024])
        w_ed = din("w_e_down", [DEPTH, NEXP, 1024, D_])
    rope_in = din("rope", [128, NT, 2, 2, 32])
    consts_in = din("consts", [128, 6, 128])
    negm_in = din("negmask", [128, 8, 8])
    ebase_in = din("ebase", [128, NEXP])
    y_out = nc.dram_tensor("y", [S_, D_], F32, kind="ExternalOutput").ap()
    dbg = {}

    def dbg_out(name, shape, dt=F32):
        dbg[name] = nc.dram_tensor(name, list(shape), dt, kind="ExternalOutput").ap()
        return dbg[name]

    xd = nc.dram_tensor("xd", [S_, D_], F32).ap()
    mixTd = nc.dram_tensor("mixTd", [KT, 128, S_], BF16).ap()
    Xg = nc.dram_tensor("Xg", [NSLOT, D_], BF16).ap()
    Yg = nc.dram_tensor("Yg", [NSLOT, D_], F32).ap()

    sb = nc.alloc_sbuf_tensor
    _uid = [0]

    def SBT(name, shape, dt):
        _uid[0] += 1
        return nc.sbuf_tensor("%s_u%d" % (name, _uid[0]), shape, dt)
    cst_f = sb("cst_f", [128, 6, 128], F32)
    cst_b = sb("cst_b", [128, 6, 128], BF16)
    ident_f, triI_f, triR_f, triS_f, ones_f = (cst_f[:, i, :] for i in range(5))
    ident_b, triI_b = cst_b[:, 0, :], cst_b[:, 1, :]
    rope = sb("rope_sb", [128, NT, 2, 2, 32], F32)
    negm = sb("negm", [128, 8, 8], F32)
    ebase = sb("ebase_sb", [128, NEXP], F32)
    lnT = sb("lnT_sb", [128, 2 * DEPTH + 1, KT], F32)
    modT = sb("modT", [128, DEPTH, 96], F32)
    g1T = sb("g1T", [128, KT], F32)
    g2T = sb("g2T", [128, KT], F32)
    wgk = sb("wgk", [17, DEPTH, 512], F32)
    gon = sb("gon", [128, DEPTH, 256], F32)
    wr = sb("wr_sb", [128, DEPTH, KT, 36], F32)
    br = sb("br_sb", [1, DEPTH, 36], F32)
    idx_a = sb("idx_a", [128, NT], I32)
    idx_b = sb("idx_b", [128, NT], I32)
    wts = sb("wts", [128, NT, 2], F32)
    selacc = sb("selacc", [128, NEXP], F32)
    sm = sb("sm", [128, 64], F32)
    cact = sb("cact", [128, KT], BF16)
    badaT = sb("badaT_sb", [128, DEPTH, 96], F32)
    ps = [nc.alloc_psum_tensor("ps%d" % i, [128, 512], F32) for i in range(8)]
    P = ['ps:%d' % i for i in range(8)]

    def smc(i, n=1):
        return sm[:, i:i + n]

    def ld(dst, src, name):
        S.dma('sp', lambda e: e.dma_start(out=dst, in_=src), reads=['dr:' + name], writes=['sb:' + name])
    ld(cst_f[:, :, :], consts_in[:, :, :], 'cst_f')
    ld(rope[:, :, :, :, :], rope_in[:, :, :, :, :], 'rope')
    ld(negm[:, :, :], negm_in[:, :, :], 'negm')
    ld(ebase[:, :], ebase_in[:, :], 'ebase')
    ld(lnT[:, :, :], lnT_in[:, :, :], 'lnT')
    ld(wgk[:, :, :], wgk_in[:, :, :], 'wgk')
    ld(gon[:, :, :], gon_in[:, :, :], 'gon')
    ld(wr[:, :, :, :], wr_in[:, :, :, :], 'wr')
    ld(br[:, :, :], br_in[:, :, :], 'br')
    S.op('dve', lambda e: e.tensor_copy(out=cst_b[:, :, :], in_=cst_f[:, :, :]), reads=['sb:cst_f'], writes=['sb:cst_b'])

    import contextlib
    with contextlib.ExitStack() as es:
        cT = es.enter_context(SBT("cT_sb", [128, KT], F32))
        wring = [es.enter_context(SBT("wa%d" % i, [128, KT, 512], BF16)) for i in range(3)]
        ld(cT[:, :], cT_in[:, :], 'cT')
        ld(badaT[:, :, :], badaT_in[:, :, :], 'badaT')
        S.op('act', lambda e: e.activation(out=cact[:, :], in_=cT[:, :], func=AF.Silu), reads=['sb:cT'], writes=['sb:cact'])
        for l in range(1):
            wv = w_ada[l].rearrange("(k p) n -> p k n", p=128)
            for pc in range(24):
                buf = wring[pc % 3]
                rn = 'sb:wa%d' % (pc % 3)
                for hh in range(2):
                    S.dma('pool', lambda e, buf=buf, pc=pc, hh=hh: e.dma_start(
                        out=buf[:, hh * 8:(hh + 1) * 8, :], in_=wv[:, hh * 8:(hh + 1) * 8, pc * 512:(pc + 1) * 512]),
                        reads=['dr:w_ada'], writes=[rn], acc=(hh == 1))
                for m in range(4):
                    j = pc * 4 + m
                    bank = (l * 96 + j) // 512
                    col = l * 96 + j
                    S.group('pe', [lambda e, buf=buf, m=m, k=k, col=col: e.matmul(
                        ps[0][:, col:col + 1], lhsT=buf[:, k, m * 128:(m + 1) * 128], rhs=cact[:, k:k + 1],
                        start=(k == 0), stop=(k == KT - 1)) for k in range(KT)],
                        reads=[rn, 'sb:cact'], writes=[P[0]])
        S.op('dve', lambda e: e.tensor_tensor(out=modT[:, 0, :], in0=ps[0][:, 0:96], in1=badaT[:, 0, :], op=ALU.add),
             reads=[P[0], 'sb:badaT'], writes=['sb:modT'])
        S.barrier()
    if debug:
        d = dbg_out("dbg_modT", [128, DEPTH, 96])
        S.dma('sp', lambda e: e.dma_start(out=d[:, :, :], in_=modT[:, :, :]), reads=['sb:modT'], writes=['dr:dbg_modT'])
    if stop_after == 'mod':
        S.finish()
        return nc, dbg

    def bcast(dst, vcols, dg, rname, vres, pbank=7):
        for kt in range(KT):
            S.op('dve', lambda e, kt=kt: e.tensor_scalar(out=dg[:, :], in0=ident_f, scalar1=vcols(kt), scalar2=None, op0=ALU.mult),
                 reads=['sb:cst_f'] + vres, writes=['sb:dg'])
            S.op('pe', lambda e, kt=kt: e.matmul(ps[pbank][:, 0:128], lhsT=ones_f, rhs=dg[:, :], start=True, stop=True),
                 reads=['sb:dg', 'sb:cst_f'], writes=[P[pbank]])
            S.op('act', lambda e, kt=kt: e.activation(out=dst[:, kt * 128:(kt + 1) * 128], in_=ps[pbank][:, 0:128], func=AF.Copy),
                 reads=[P[pbank]], writes=[rname])

    def rstd_from_ss(ss_col, out_col, n, tag):
        S.op('act', lambda e: e.activation(out=out_col, in_=ss_col, func=AF.Ln, scale=1.0 / n, bias=eps_t[:, 0:1]),
             reads=['sb:' + tag + '_ss', 'sb:eps'], writes=['sb:' + tag + '_sq'])
        S.op('act', lambda e: e.activation(out=out_col, in_=out_col, func=AF.Exp, scale=-0.5), reads=['sb:' + tag + '_sq'], writes=['sb:' + tag + '_rs'])

    eps_t = sb("eps_t", [128, 1], F32)
    bc_reg = nc.gpsimd.to_reg(NSLOT - 1)
    S.op('pool', lambda e: e.memset(eps_t[:, :], EPS), writes=['sb:eps'])

    for l in range(n_layers):
        x_src = x_in if l == 0 else xd
        xres = 'dr:x_in' if l == 0 else 'dr:xd'
        last = (l == n_layers - 1)
        sh1 = lambda kt, l=l: modT[:, l, 0 + kt:0 + kt + 1]
        sc1 = lambda l=l: modT[:, l, 16:32]
        gt1 = lambda kt, l=l: modT[:, l, 32 + kt:32 + kt + 1]
        sh2 = lambda kt, l=l: modT[:, l, 48 + kt:48 + kt + 1]
        sc2 = lambda l=l: modT[:, l, 64:80]
        gt2 = lambda kt, l=l: modT[:, l, 80 + kt:80 + kt + 1]
        S.op('dve', lambda e: e.scalar_tensor_tensor(out=g1T[:, :], in0=sc1(), scalar=1.0, in1=lnT[:, 2 * l, :], op0=ALU.add, op1=ALU.mult),
             reads=['sb:modT', 'sb:lnT'], writes=['sb:g1T'])
        S.op('dve', lambda e: e.scalar_tensor_tensor(out=g2T[:, :], in0=sc2(), scalar=1.0, in1=lnT[:, 2 * l + 1, :], op0=ALU.add, op1=ALU.mult),
             reads=['sb:modT', 'sb:lnT'], writes=['sb:g2T'])

        with contextlib.ExitStack() as esL:
            hT = esL.enter_context(SBT("hT", [128, KT, S_], BF16))
            with contextlib.ExitStack() as es:
                xt = [es.enter_context(SBT("xt%d" % i, [128, D_], F32)) for i in range(2)]
                junk = es.enter_context(SBT("junk", [128, D_], BF16))
                for i in range(NT):
                    xb = xt[i % 2]
                    xn_ = 'sb:xt%d' % (i % 2)
                    S.dma('sp', lambda e, xb=xb, i=i: e.dma_start(out=xb[:, :], in_=x_src[i * 128:(i + 1) * 128, :]),
                          reads=[xres], writes=[xn_])
                    S.op('act', lambda e, xb=xb: e.activation(out=junk[:, :], in_=xb[:, :], func=AF.Square, accum_out=smc(0)),
                         reads=[xn_], writes=['sb:junk', 'sb:n1_ss'])
                    rstd_from_ss(smc(0), smc(1), D_, 'n1')
                    S.op('act', lambda e, xb=xb: e.activation(out=xb[:, :], in_=xb[:, :], func=AF.Copy, scale=smc(1)),
                         reads=[xn_, 'sb:n1_rs'], writes=[xn_])
                    for g4 in range(4):
                        bank = g4 % 4
                        S.group('pe', [lambda e, xb=xb, kt=kt, bank=bank: e.transpose(
                            ps[bank][:, (kt % 4) * 128:(kt % 4 + 1) * 128], xb[:, kt * 128:(kt + 1) * 128], ident_f)
                            for kt in range(g4 * 4, g4 * 4 + 4)], reads=[xn_, 'sb:cst_f'], writes=[P[bank]])
                        for kt in range(g4 * 4, g4 * 4 + 4):
                            eng = 'dve' if kt % 2 == 0 else 'dve'
                            S.op(eng, lambda e, kt=kt, bank=bank, i=i: e.tensor_scalar(
                                out=hT[:, kt, i * 128:(i + 1) * 128], in0=ps[bank][:, (kt % 4) * 128:(kt % 4 + 1) * 128],
                                scalar1=g1T[:, kt:kt + 1], scalar2=sh1(kt), op0=ALU.mult, op1=ALU.add),
                                reads=[P[bank], 'sb:g1T', 'sb:modT'], writes=['sb:hT'])
                S.barrier()
            if debug and l == 0:
                d = dbg_out("dbg_hT", [KT, 128, S_], BF16)
                S.dma('sp', lambda e: e.dma_start(out=d.rearrange("k p s -> p k s"), in_=hT[:, :, :]), reads=['sb:hT'], writes=['dr:dbg_hT'])
            if stop_after == 'B':
                S.finish()
                return nc, dbg

            wl = w_in[l].rearrange("(k p) n -> p k n", p=128)
            with contextlib.ExitStack() as es:
                wA = [es.enter_context(SBT("wA%d" % i, [128, KT, 384], BF16)) for i in range(2)]
                tA2 = [es.enter_context(SBT("tA%d" % z, [128, 2, 32], F32)) for z in range(2)]
                tB2 = [es.enter_context(SBT("tB%d" % z, [128, 2, 32], F32)) for z in range(2)]
                qkb2 = [es.enter_context(SBT("qkb%d" % z, [128, 2, 128], BF16)) for z in range(2)]
                qkT = es.enter_context(SBT("qkT", [128, 2, S_], BF16))
                vS = es.enter_context(SBT("vS", [128, NT, 132], BF16))
                kmf = es.enter_context(SBT("kmf", [128, 8], F32))
                kmb = es.enter_context(SBT("kmb", [128, 8], BF16))
                gm = es.enter_context(SBT("gm", [128, 8], F32))
                top8 = es.enter_context(SBT("top8", [128, 8], F32))
                sel = es.enter_context(SBT("sel", [128, NT, 8], F32))
                pT = [es.enter_context(SBT("pT%d" % i, [128, 512], BF16)) for i in range(4)]
                oacc = es.enter_context(SBT("oacc", [128, NT, 132], F32))
                orec = es.enter_context(SBT("orec", [128, NT], F32))
                oab = [es.enter_context(SBT("oab%d" % i, [128, 128], BF16)) for i in range(2)]
                oT = [es.enter_context(SBT("oT%d" % i, [128, S_], BF16)) for i in range(2)]
                nxt = (l + 1 < n_layers)
                if nxt:
                    wring2 = [es.enter_context(SBT("wb%d" % i, [128, KT, 512], BF16)) for i in range(3)]
                    wv2 = w_ada[l + 1].rearrange("(k p) n -> p k n", p=128)

                def ada_piece(pc):
                    bufa = wring2[pc % 3]
                    rn = 'sb:wb%d' % (pc % 3)
                    for hh in range(2):
                        S.dma('pool', lambda e, hh=hh: e.dma_start(
                            out=bufa[:, hh * 8:(hh + 1) * 8, :], in_=wv2[:, hh * 8:(hh + 1) * 8, pc * 512:(pc + 1) * 512]),
                            reads=['dr:w_ada'], writes=[rn], acc=(hh == 1))
                    for m in range(4):
                        S.group('pe', [lambda e, m=m, k=k: e.matmul(
                            ps[3][:, 100 + m:101 + m], lhsT=bufa[:, k, m * 128:(m + 1) * 128], rhs=cact[:, k:k + 1],
                            start=(k == 0), stop=(k == KT - 1)) for k in range(KT)],
                            reads=[rn, 'sb:cact'], writes=[P[3]])
                    S.op('dve', lambda e: e.tensor_tensor(out=modT[:, l + 1, 4 * pc:4 * pc + 4], in0=ps[3][:, 100:104], in1=badaT[:, l + 1, 4 * pc:4 * pc + 4], op=ALU.add),
                         reads=[P[3], 'sb:badaT'], writes=['sb:modT_next'])
                if l == 0:
                    zer = es.enter_context(SBT("zer", [128, 2, D_], BF16))
                    S.op('pool', lambda e: e.memset(zer[:, :, :], 0.0), writes=['sb:zer'])
                    Xgv = Xg.rearrange("(n p) d -> p n d", p=128)
                    for z in range(NSLOT // 256):
                        S.dma('sp', lambda e, z=z: e.dma_start(out=Xgv[:, 2 * z:2 * z + 2, :], in_=zer[:, :, :]), reads=['sb:zer'], writes=['dr:Xg'], acc=True)
                S.op('pool', lambda e: e.memset(vS[:, :, :], 1.0), writes=['sb:vS'])
                S.op('pool', lambda e: e.memset(sel[:, :, :], 1.0), writes=['sb:sel'])

                def load_wA(h):
                    buf = wA[h % 2]
                    for j, c0 in enumerate([h * 128, 1024 + h * 128, 2048 + h * 128]):
                        S.dma('pool', lambda e, buf=buf, j=j, c0=c0: e.dma_start(
                            out=buf[:, :, j * 128:(j + 1) * 128], in_=wl[:, :, c0:c0 + 128]),
                            reads=['dr:w_in'], writes=['sb:wA%d' % (h % 2)], acc=(j > 0))
                if not DBG['skip_c1']:
                    load_wA(0)
                for h in range(0 if DBG['skip_c1'] else 8):
                    if h + 1 < 8:
                        load_wA(h + 1)
                    buf = wA[h % 2]
                    wn = 'sb:wA%d' % (h % 2)
                    def c1_mm(i):
                        pb = i % 2
                        S.group('pe', [lambda e, k=k, i=i, pb=pb: e.matmul(
                            ps[pb][:, 0:384], lhsT=hT[:, k, i * 128:(i + 1) * 128], rhs=buf[:, k, :],
                            start=(k == 0), stop=(k == KT - 1)) for k in range(KT)],
                            reads=['sb:hT', wn], writes=[P[pb]])

                    def c1_chain(i):
                        tA, tB, qkb = tA2[i % 2], tB2[i % 2], qkb2[i % 2]
                        pz = str(i % 2)
                        pb = i % 2
                        psv = ps[pb][:, 0:256].rearrange("p (a d) -> p a d", a=2)
                        cs = rope[:, i, 0, :, :]
                        sn = rope[:, i, 1, :, :]
                        S.op('dve', lambda e: e.tensor_tensor(out=tA[:, :, :], in0=psv[:, :, 0:32], in1=cs, op=ALU.mult),
                             reads=[P[pb], 'sb:rope'], writes=['sb:tA' + pz])
                        S.op('dve', lambda e: e.tensor_tensor(out=tB[:, :, 0:16], in0=psv[:, :, 16:32], in1=sn[:, :, 0:16], op=ALU.mult),
                             reads=[P[pb], 'sb:rope'], writes=['sb:tB' + pz])
                        S.op('dve', lambda e: e.tensor_tensor(out=tB[:, :, 16:32], in0=psv[:, :, 0:16], in1=sn[:, :, 16:32], op=ALU.mult),
                             reads=[P[pb], 'sb:rope'], writes=['sb:tB' + pz])
                        S.op('act', lambda e: e.activation(out=qkb[:, 0, 32:128], in_=ps[pb][:, 32:128], func=AF.Copy, scale=ATTN_SCALE),
                             reads=[P[pb]], writes=['sb:qkb' + pz])
                        S.op('act', lambda e: e.activation(out=qkb[:, 1, 32:128], in_=ps[pb][:, 160:256], func=AF.Copy),
                             reads=[P[pb]], writes=['sb:qkb' + pz])
                        S.op('act', lambda e: e.activation(out=vS[:, i, 0:128], in_=ps[pb][:, 256:384], func=AF.Copy),
                             reads=[P[pb]], writes=['sb:vS'])
                        S.op('dve', lambda e: e.tensor_tensor(out=qkb[:, :, 0:32], in0=tA[:, :, :], in1=tB[:, :, :], op=ALU.add),
                             reads=['sb:tA' + pz, 'sb:tB' + pz], writes=['sb:qkb' + pz])

                    def c1_tr(i):
                        qkb = qkb2[i % 2]
                        pz = str(i % 2)
                        tps = ps[2][:, 0:128].bitcast(BF16)
                        S.group('pe', [lambda e, a=a: e.transpose(tps[:, a * 128:(a + 1) * 128], qkb[:, a, :], ident_b) for a in range(2)],
                                reads=['sb:qkb' + pz, 'sb:cst_b'], writes=[P[2]])
                        S.op('dve', lambda e: e.tensor_copy(out=qkT[:, :, i * 128:(i + 1) * 128],
                                                            in_=tps.rearrange("p (a t) -> p a t", a=2)),
                             reads=[P[2]], writes=['sb:qkT'])
                    c1_mm(0)
                    for i in range(NT):
                        c1_chain(i)
                        if i + 1 < NT:
                            c1_mm(i + 1)
                        c1_tr(i)
                        if nxt and i in (3, 8, 13):
                            ada_piece(3 * h + (i - 3) // 5)
                    S.op('dve', lambda e: e.tensor_reduce(out=kmf[:, :], in_=qkT[:, 1, :].rearrange("p (n s) -> p n s", n=8), axis=AX.X, op=ALU.add),
                         reads=['sb:qkT'], writes=['sb:kmf'])
                    S.op('act', lambda e: e.activation(out=kmb[:, :], in_=kmf[:, :], func=AF.Copy, scale=1.0 / 256.0),
                         reads=['sb:kmf'], writes=['sb:kmb'])
                    for c in range(8, NT):
                        blk = c // 2
                        S.op('pe', lambda e, c=c: e.matmul(ps[3][:, 0:8], lhsT=qkT[:, 0, c * 128:(c + 1) * 128], rhs=kmb[:, :], start=True, stop=True),
                             reads=['sb:qkT', 'sb:kmb'], writes=[P[3]])
                        S.op('dve', lambda e, blk=blk: e.tensor_tensor(out=gm[:, :], in0=ps[3][:, 0:8], in1=negm[:, blk, :], op=ALU.add),
                             reads=[P[3], 'sb:negm'], writes=['sb:gm'])
                        S.op('dve', lambda e: e.max(out=top8[:, :], in_=gm[:, :]), reads=['sb:gm'], writes=['sb:top8'])
                        S.op('dve', lambda e, c=c: e.tensor_scalar(out=sel[:, c, :], in0=gm[:, :], scalar1=top8[:, 2:3], scalar2=None, op0=ALU.is_ge),
                             reads=['sb:gm', 'sb:top8'], writes=['sb:sel'])
                    ob_ = oT[h % 2]
                    on_ = 'sb:oT%d' % (h % 2)
                    S.op('pool', lambda e: e.memset(oacc[:, :, :], 0.0), writes=['sb:oacc'])
                    its = []
                    for b in range(8):
                        for s4 in range((2 * b) // 4, 4):
                            its.append((b, s4))
                    ocnt = [0]

                    def emit_scores(ii):
                        b, s4 = its[ii]
                        info = []
                        for jj in range(2):
                            j = 2 * b + jj
                            c0 = max(4 * s4, j)
                            nq = 4 * s4 + 4 - c0
                            if nq <= 0:
                                info.append(None)
                                continue
                            sbk = 4 + 2 * (ii % 2) + jj
                            pt = pT[2 * (ii % 2) + jj]
                            pn = 'sb:pT%d' % (2 * (ii % 2) + jj)
                            S.op('pe', lambda e, j=j, c0=c0, nq=nq, sbk=sbk: e.matmul(
                                ps[sbk][:, 0:nq * 128], lhsT=qkT[:, 1, j * 128:(j + 1) * 128], rhs=qkT[:, 0, c0 * 128:(c0 + nq) * 128],
                                start=True, stop=True), reads=['sb:qkT'], writes=[P[sbk]])
                            S.op('act', lambda e, sbk=sbk, nq=nq, pt=pt: e.activation(out=pt[:, 0:nq * 128], in_=ps[sbk][:, 0:nq * 128], func=AF.Exp),
                                 reads=[P[sbk]], writes=[pn])
                            if c0 == j:
                                S.op('pool', lambda e, pt=pt: e.tensor_tensor(out=pt[:, 0:128], in0=pt[:, 0:128], in1=triI_b, op=ALU.mult),
                                     reads=[pn, 'sb:cst_b'], writes=[pn])
                            info.append((j, c0, nq, pt, pn))
                        return info

                    def emit_pv(ii, info):
                        b, s4 = its[ii]
                        for c in range(max(4 * s4, 2 * b), 4 * s4 + 4):
                            own = (c // 2 == b)
                            parts = []
                            for t in info:
                                if t is None:
                                    continue
                                j, c0, nq, pt, pn = t
                                if c < c0 or j > c:
                                    continue
                                parts.append((j, pt, pn, (c - c0) * 128))
                            if not parts:
                                continue
                            ob_i = ocnt[0] % 4
                            ocnt[0] += 1
                            obk = ob_i
                            oc0 = 0
                            S.group('pe', [lambda e, j=j, pt=pt, off=off, obk=obk, oc0=oc0, first=(pi_ == 0), lastp=(pi_ == len(parts) - 1): e.matmul(
                                ps[obk][:, oc0:oc0 + 129], lhsT=pt[:, off:off + 128], rhs=vS[:, j, 0:129], start=first, stop=lastp)
                                for pi_, (j, pt, pn, off) in enumerate(parts)], reads=[p[2] for p in parts] + ['sb:vS'], writes=[P[obk]])
                            if own:
                                S.op('dve', lambda e, obk=obk, oc0=oc0, c=c: e.tensor_tensor(
                                    out=oacc[:, c, 0:129], in0=ps[obk][:, oc0:oc0 + 129], in1=oacc[:, c, 0:129], op=ALU.add),
                                    reads=[P[obk], 'sb:oacc'], writes=['sb:oacc'])
                            else:
                                S.op('dve', lambda e, obk=obk, oc0=oc0, c=c, b=b: e.scalar_tensor_tensor(
                                    out=oacc[:, c, 0:129], in0=ps[obk][:, oc0:oc0 + 129], scalar=sel[:, c, b:b + 1], in1=oacc[:, c, 0:129],
                                    op0=ALU.mult, op1=ALU.add), reads=[P[obk], 'sb:sel', 'sb:oacc'], writes=['sb:oacc'])
                    prev = None
                    for ii in range(len(its)):
                        inf = emit_scores(ii)
                        if prev is not None:
                            emit_pv(*prev)
                        prev = (ii, inf)
                    emit_pv(*prev)
                    S.op('dve', lambda e: e.reciprocal(out=orec[:, :], in_=oacc[:, :, 128]), reads=['sb:oacc'], writes=['sb:orec'])
                    for c in range(NT):
                        S.op('dve' if c % 2 == 0 else 'pool', lambda e, c=c: e.tensor_scalar(out=oab[c % 2][:, :], in0=oacc[:, c, 0:128], scalar1=orec[:, c:c + 1], scalar2=None, op0=ALU.mult),
                             reads=['sb:oacc', 'sb:orec'], writes=['sb:oab%d' % (c % 2)])
                        tps = ps[2 + c % 2][:, 0:64].bitcast(BF16)
                        S.op('pe', lambda e, c=c, tps=tps: e.transpose(tps, oab[c % 2][:, :], ident_b), reads=['sb:oab%d' % (c % 2), 'sb:cst_b'], writes=[P[2 + c % 2]])
                        S.op('act', lambda e, c=c, tps=tps: e.activation(out=ob_[:, c * 128:(c + 1) * 128], in_=tps, func=AF.Copy),
                             reads=[P[2 + c % 2]], writes=[on_])
                    S.dma('sp', lambda e, h=h: e.dma_start(out=mixTd[h], in_=ob_[:, :]), reads=[on_], writes=['dr:mixTd'], acc=True)
                S.barrier()
            if stop_after == 'C1':
                if debug:
                    d = dbg_out("dbg_mixT", [KT, 128, S_], BF16)
                    S.dma('sp', lambda e: e.dma_start(out=d[0:8], in_=mixTd[0:8]), reads=['dr:mixTd'], writes=['dr:dbg_mixT'])
                S.finish()
                return nc, dbg

            with contextlib.ExitStack() as es:
                wG = [es.enter_context(SBT("wG%d" % i, [128, KT, 784], BF16)) for i in range(2)]
                gaA2 = [es.enter_context(SBT("gaA%d" % z, [128, 32], F32)) for z in range(2)]
                gaT2 = [es.enter_context(SBT("gaT%d" % z, [32, 128], F32)) for z in range(2)]
                lsp2 = [es.enter_context(SBT("lsp%d" % z, [128, 128], F32)) for z in range(2)]
                E12 = [es.enter_context(SBT("E1%d" % z, [128, 128], F32)) for z in range(2)]
                E22 = [es.enter_context(SBT("E2%d" % z, [128, 128], F32)) for z in range(2)]
                E32 = [es.enter_context(SBT("E3%d" % z, [128, 128], F32)) for z in range(2)]
                qdki2 = [es.enter_context(SBT("qdki%d" % z, [128, 2, 128], BF16)) for z in range(2)]
                keb2 = [es.enter_context(SBT("keb%d" % z, [128, 128], BF16)) for z in range(2)]
                qkTg2 = [es.enter_context(SBT("qkTg%d" % z, [128, 2, 128], BF16)) for z in range(2)]
                vb2 = [es.enter_context(SBT("vb%d" % z, [128, 256], BF16)) for z in range(2)]
                sg2 = [es.enter_context(SBT("sg%d" % z, [128, 256], F32)) for z in range(2)]
                attb2 = [es.enter_context(SBT("attb%d" % z, [128, 128], BF16)) for z in range(2)]
                Sf = es.enter_context(SBT("Sf", [128, 256], F32))
                Sb = [es.enter_context(SBT("Sb%d" % i, [128, 256], BF16)) for i in range(2)]
                tmpo2 = [es.enter_context(SBT("tmpo%d" % z, [128, 256], F32)) for z in range(2)]
                obb2 = [es.enter_context(SBT("obb%d" % z, [128, 256], BF16)) for z in range(2)]
                junk22 = [es.enter_context(SBT("junk2%d" % z, [128, 256], BF16)) for z in range(2)]
                oT2 = [es.enter_context(SBT("oT2_%d" % i, [128, 2, S_], BF16)) for i in range(2)]
                for z in range(2):
                    S.op('pool', lambda e, z=z: e.memset(gaA2[z][:, :], 1.0), writes=['sb:gaA%d' % z])

                NTG = DBG['gla_tiles']

                def load_wG(h):
                    buf = wG[h % 2]
                    segs = [(3072 + h * 128, 128, 0), (3584 + h * 128, 128, 128), (4096 + h * 256, 256, 256),
                            (5120 + h * 256, 256, 512), (6144, 16, 768)]
                    for j, (c0, n, o0) in enumerate(segs):
                        S.dma('pool', lambda e, buf=buf, c0=c0, n=n, o0=o0: e.dma_start(
                            out=buf[:, :, o0:o0 + n], in_=wl[:, :, c0:c0 + n]),
                            reads=['dr:w_in'], writes=['sb:wG%d' % (h % 2)], acc=(j > 0))
                load_wG(0)
                for h in range(DBG['gla_heads']):
                    if h + 1 < DBG['gla_heads']:
                        load_wG(h + 1)
                    buf = wG[h % 2]
                    wn = 'sb:wG%d' % (h % 2)
                    o2 = oT2[h % 2]
                    o2n = 'sb:oT2_%d' % (h % 2)
                    def gla_in(h, i, buf=buf, wn=wn, o2=o2, o2n=o2n):
                            gaA, gaT, lsp, E1, E2, E3, qdki, keb, qkTg, vb, sg, attb, tmpo, obb, junk2 = gaA2[i % 2], gaT2[i % 2], lsp2[i % 2], E12[i % 2], E22[i % 2], E32[i % 2], qdki2[i % 2], keb2[i % 2], qkTg2[i % 2], vb2[i % 2], sg2[i % 2], attb2[i % 2], tmpo2[i % 2], obb2[i % 2], junk22[i % 2]
                            pz = str(i % 2)
                            smc = lambda kk, n=1: sm[:, kk + 26 * (i % 2):kk + 26 * (i % 2) + n]
                            pA = [0, 2][i % 2]
                            pB = [1, 7][i % 2]
                            S.group('pe', [lambda e, k=k, i=i: e.matmul(ps[pA][:, 0:512], lhsT=hT[:, k, i * 128:(i + 1) * 128], rhs=buf[:, k, 0:512],
                                                                        start=(k == 0), stop=(k == KT - 1)) for k in range(KT)],
                                    reads=['sb:hT', wn], writes=[P[pA]])
                            S.group('pe', [lambda e, k=k, i=i: e.matmul(ps[pB][:, 0:272], lhsT=hT[:, k, i * 128:(i + 1) * 128], rhs=buf[:, k, 512:784],
                                                                        start=(k == 0), stop=(k == KT - 1)) for k in range(KT)],
                                    reads=['sb:hT', wn], writes=[P[pB]])
                    def gla_mid(h, i, buf=buf, wn=wn, o2=o2, o2n=o2n):
                            gaA, gaT, lsp, E1, E2, E3, qdki, keb, qkTg, vb, sg, attb, tmpo, obb, junk2 = gaA2[i % 2], gaT2[i % 2], lsp2[i % 2], E12[i % 2], E22[i % 2], E32[i % 2], qdki2[i % 2], keb2[i % 2], qkTg2[i % 2], vb2[i % 2], sg2[i % 2], attb2[i % 2], tmpo2[i % 2], obb2[i % 2], junk22[i % 2]
                            pz = str(i % 2)
                            smc = lambda kk, n=1: sm[:, kk + 26 * (i % 2):kk + 26 * (i % 2) + n]
                            pA = [0, 2][i % 2]
                            pB = [1, 7][i % 2]
                            S.op('act', lambda e: e.activation(out=gaA[:, 0:16], in_=ps[pB][:, 256:272], func=AF.Copy), reads=[P[pB]], writes=['sb:gaA' + pz])
                            S.op('pe', lambda e: e.transpose(ps[4][0:32, 128:256], gaA[:, 0:32], ident_f), reads=['sb:gaA' + pz, 'sb:cst_f'], writes=[P[4]])
                            S.op('act', lambda e: e.activation(out=gaT[:, :], in_=ps[4][0:32, 128:256], func=AF.Copy), reads=[P[4]], writes=['sb:gaT' + pz])
                            S.op('pe', lambda e, h=h: e.matmul(ps[3][:, 0:128], lhsT=gaT[0:17, :], rhs=wgk[0:17, l, h * 128:(h + 1) * 128], start=True, stop=True),
                                 reads=['sb:gaT' + pz, 'sb:wgk'], writes=[P[3]])
                            S.op('act', lambda e: e.activation(out=lsp[:, :], in_=ps[3][:, 0:128], func=AF.Exp, scale=-1.0), reads=[P[3]], writes=['sb:lsp' + pz])
                            S.op('act', lambda e: e.activation(out=lsp[:, :], in_=lsp[:, :], func=AF.Ln, bias=1.0), reads=['sb:lsp' + pz], writes=['sb:lsp' + pz])
                            S.op('pe', lambda e: e.matmul(ps[3][:, 128:256], lhsT=triI_f, rhs=lsp[:, :], start=True, stop=True),
                                 reads=['sb:lsp' + pz, 'sb:cst_f'], writes=[P[3]])
                            S.op('pe', lambda e: e.matmul(ps[3][:, 256:384], lhsT=triR_f, rhs=lsp[:, :], start=True, stop=True),
                                 reads=['sb:lsp' + pz, 'sb:cst_f'], writes=[P[3]])
                            S.op('pe', lambda e: e.matmul(ps[3][:, 384:385], lhsT=lsp[:, :], rhs=ones_f[:, 0:1], start=True, stop=True),
                                 reads=['sb:lsp' + pz, 'sb:cst_f'], writes=[P[3]])
                            if i + 1 < NTG:
                                gla_in(h, i + 1)
                            gs = 1.0 / 16.0
                            S.op('act', lambda e: e.activation(out=E1[:, :], in_=ps[3][:, 128:256], func=AF.Exp, scale=-gs), reads=[P[3]], writes=['sb:E1' + pz])
                            S.op('act', lambda e: e.activation(out=E2[:, :], in_=ps[3][:, 128:256], func=AF.Exp, scale=gs), reads=[P[3]], writes=['sb:E2' + pz])
                            S.op('act', lambda e: e.activation(out=E3[:, :], in_=ps[3][:, 256:384], func=AF.Exp, scale=-gs), reads=[P[3]], writes=['sb:E3' + pz])
                            S.op('act', lambda e: e.activation(out=smc(4), in_=ps[3][:, 384:385], func=AF.Exp, scale=-gs), reads=[P[3]], writes=['sb:dec' + pz])
                            S.op('dve', lambda e: e.scalar_tensor_tensor(out=qdki[:, 0, :], in0=ps[pA][:, 0:128], scalar=GK_SCALE, in1=E1[:, :], op0=ALU.mult, op1=ALU.mult),
                                 reads=[P[pA], 'sb:E1' + pz], writes=['sb:qdki' + pz])
                            S.op('dve', lambda e: e.tensor_tensor(out=qdki[:, 1, :], in0=ps[pA][:, 128:256], in1=E2[:, :], op=ALU.mult),
                                 reads=[P[pA], 'sb:E2' + pz], writes=['sb:qdki' + pz])
                            S.op('dve', lambda e: e.tensor_tensor(out=keb[:, :], in0=ps[pA][:, 128:256], in1=E3[:, :], op=ALU.mult),
                                 reads=[P[pA], 'sb:E3' + pz], writes=['sb:keb' + pz])
                            S.op('act', lambda e: e.activation(out=vb[:, :], in_=ps[pA][:, 256:512], func=AF.Copy), reads=[P[pA]], writes=['sb:vb' + pz])
                            tps = ps[4][:, 0:128].bitcast(BF16)
                            S.group('pe', [lambda e, a=a: e.transpose(tps[:, a * 128:(a + 1) * 128], qdki[:, a, :], ident_b) for a in range(2)],
                                    reads=['sb:qdki' + pz, 'sb:cst_b'], writes=[P[4]])
                            S.op('dve', lambda e: e.tensor_copy(out=qkTg[:, :, :], in_=tps.rearrange("p (a t) -> p a t", a=2)), reads=[P[4]], writes=['sb:qkTg' + pz])
                            S.op('pe', lambda e: e.matmul(ps[5][:, 0:128], lhsT=qkTg[:, 1, :], rhs=qkTg[:, 0, :], start=True, stop=True),
                                 reads=['sb:qkTg' + pz], writes=[P[5]])
                            S.op('dve', lambda e: e.tensor_tensor(out=attb[:, :], in0=ps[5][:, 0:128], in1=triI_f, op=ALU.mult),
                                 reads=[P[5], 'sb:cst_f'], writes=['sb:attb' + pz])
                            sprev = Sb[(i + 1) % 2]
                            spn = 'sb:Sb%d' % ((i + 1) % 2)
                            scur = Sb[i % 2]
                            scn = 'sb:Sb%d' % (i % 2)
                            mm = [lambda e: e.matmul(ps[6][:, 0:256], lhsT=attb[:, :], rhs=vb[:, :], start=True, stop=(i == 0))]
                            rd = ['sb:attb' + pz, 'sb:vb' + pz]
                            if i > 0:
                                mm.append(lambda e, sprev=sprev: e.matmul(ps[6][:, 0:256], lhsT=qkTg[:, 0, :], rhs=sprev[:, :], start=False, stop=True))
                                rd += ['sb:qkTg' + pz, spn]
                            S.group('pe', mm, reads=rd, writes=[P[6]])
                            S.op('pe', lambda e: e.matmul(ps[5][:, 128:384], lhsT=keb[:, :], rhs=vb[:, :], start=True, stop=True),
                                 reads=['sb:keb' + pz, 'sb:vb' + pz], writes=[P[5]])
                            if i == 0:
                                S.op('dve', lambda e: e.tensor_copy(out=Sf[:, :], in_=ps[5][:, 128:384]), reads=[P[5]], writes=['sb:Sf'])
                            else:
                                S.op('dve', lambda e: e.scalar_tensor_tensor(out=Sf[:, :], in0=Sf[:, :], scalar=smc(4), in1=ps[5][:, 128:384], op0=ALU.mult, op1=ALU.add),
                                     reads=['sb:Sf', 'sb:dec' + pz, P[5]], writes=['sb:Sf'])
                            S.op('dve', lambda e, scur=scur: e.tensor_copy(out=scur[:, :], in_=Sf[:, :]), reads=['sb:Sf'], writes=[scn])
                            S.op('act', lambda e: e.activation(out=sg[:, :], in_=ps[pB][:, 0:256], func=AF.Exp, scale=-1.0), reads=[P[pB]], writes=['sb:sg' + pz])
                            S.op('dve', lambda e: e.tensor_scalar(out=sg[:, :], in0=sg[:, :], scalar1=1.0, scalar2=None, op0=ALU.add), reads=['sb:sg' + pz], writes=['sb:sg' + pz])
                            S.op('dve', lambda e: e.reciprocal(out=sg[:, :], in_=sg[:, :]), reads=['sb:sg' + pz], writes=['sb:sg' + pz])
                            S.op('dve', lambda e: e.tensor_tensor(out=sg[:, :], in0=ps[pB][:, 0:256], in1=sg[:, :], op=ALU.mult), reads=[P[pB], 'sb:sg' + pz], writes=['sb:sg' + pz])
                            S.op('act', lambda e: e.activation(out=junk2[:, :], in_=ps[6][:, 0:256], func=AF.Square, accum_out=smc(5)),
                                 reads=[P[6]], writes=['sb:junk2' + pz, 'sb:go' + pz + '_ss'])
                            rstd_from_ss(smc(5), smc(6), 256, 'go' + pz)
                            S.op('dve', lambda e: e.scalar_tensor_tensor(out=tmpo[:, :], in0=ps[6][:, 0:256], scalar=smc(6), in1=gon[:, l, :], op0=ALU.mult, op1=ALU.mult),
                                 reads=[P[6], 'sb:go' + pz + '_rs', 'sb:gon'], writes=['sb:tmpo' + pz])
                            S.op('dve', lambda e: e.tensor_tensor(out=obb[:, :], in0=tmpo[:, :], in1=sg[:, :], op=ALU.mult),
                                 reads=['sb:tmpo' + pz, 'sb:sg' + pz], writes=['sb:obb' + pz])
                    def gla_out(h, i, buf=buf, wn=wn, o2=o2, o2n=o2n):
                            gaA, gaT, lsp, E1, E2, E3, qdki, keb, qkTg, vb, sg, attb, tmpo, obb, junk2 = gaA2[i % 2], gaT2[i % 2], lsp2[i % 2], E12[i % 2], E22[i % 2], E32[i % 2], qdki2[i % 2], keb2[i % 2], qkTg2[i % 2], vb2[i % 2], sg2[i % 2], attb2[i % 2], tmpo2[i % 2], obb2[i % 2], junk22[i % 2]
                            pz = str(i % 2)
                            smc = lambda kk, n=1: sm[:, kk + 26 * (i % 2):kk + 26 * (i % 2) + n]
                            pA = [0, 2][i % 2]
                            pB = [1, 7][i % 2]
                            tp2 = ps[4][:, 256:384].bitcast(BF16)
                            S.group('pe', [lambda e, a=a: e.transpose(tp2[:, a * 128:(a + 1) * 128], obb[:, a * 128:(a + 1) * 128], ident_b) for a in range(2)],
                                    reads=['sb:obb' + pz, 'sb:cst_b'], writes=[P[4]])
                            S.op('act', lambda e, i=i: e.activation(out=o2[:, :, i * 128:(i + 1) * 128], in_=tp2.rearrange("p (a t) -> p a t", a=2), func=AF.Copy),
                                 reads=[P[4]], writes=[o2n])
                    gla_in(h, 0)
                    for i in range(NTG):
                        gla_mid(h, i)
                        gla_out(h, i)
                    for a in range(2):
                        S.dma('sp', lambda e, a=a, h=h: e.dma_start(out=mixTd[8 + 2 * h + a], in_=o2[:, a, :]), reads=[o2n], writes=['dr:mixTd'],
                              slot='st_oT2_%d' % (h % 2), acc=True)
                S.barrier()
        if stop_after == 'C2':
            if debug:
                d = dbg_out("dbg_mixT", [KT, 128, S_], BF16)
                S.dma('sp', lambda e: e.dma_start(out=d[:, :, :], in_=mixTd[:, :, :]), reads=['dr:mixTd'], writes=['dr:dbg_mixT'])
            S.finish()
            return nc, dbg

        with contextlib.ExitStack() as esD:
            gt2b = esD.enter_context(SBT("gt2b", [128, D_], F32))
            dg = esD.enter_context(SBT("dg", [128, 128], F32))
            with contextlib.ExitStack() as es:
                mts = [es.enter_context(SBT("mt%d" % i, [128, KT, 128], BF16)) for i in range(2)]
                wo = es.enter_context(SBT("wo", [128, KT, D_], BF16))
                g2b = es.enter_context(SBT("g2b", [128, D_], F32))
                sh2b = es.enter_context(SBT("sh2b", [128, D_], F32))
                xt = [es.enter_context(SBT("xtd%d" % i, [128, D_], F32)) for i in range(2)]
                tmpx = es.enter_context(SBT("tmpx", [128, D_], F32))
                h2f2 = [es.enter_context(SBT("h2f%d" % z, [128, D_], F32)) for z in range(2)]
                h2b = [es.enter_context(SBT("h2b%d" % i, [128, D_], BF16)) for i in range(2)]
                h2T2 = [es.enter_context(SBT("h2T%d" % z, [128, KT, 128], F32)) for z in range(2)]
                junk = es.enter_context(SBT("junkd", [128, D_], BF16))
                lg2 = [es.enter_context(SBT("lg%d" % z, [128, 36], F32)) for z in range(2)]
                rt2 = [es.enter_context(SBT("rt%d" % z, [128, 8, 32], F32)) for z in range(2)]
                wov = w_out[l].rearrange("(k p) n -> p k n", p=128)
                for q4 in range(8):
                    for ch in range(2):
                        S.dma('pool', lambda e, q4=q4, ch=ch: e.dma_start(out=wo[:, q4 * 2:(q4 + 1) * 2, ch * 1024:(ch + 1) * 1024],
                                                                         in_=wov[:, q4 * 2:(q4 + 1) * 2, ch * 1024:(ch + 1) * 1024]),
                              reads=['dr:w_out'], writes=['sb:wo'], acc=(q4 + ch > 0))
                bcast(tmpx, gt1, dg, 'sb:tmpx0', ['sb:modT'])
                for k in range(KT):
                    S.op('dve' if k % 2 == 0 else 'pool', lambda e, k=k: e.tensor_tensor(out=wo[:, k, :], in0=wo[:, k, :], in1=tmpx[:, :], op=ALU.mult),
                         reads=['sb:wo', 'sb:tmpx0'], writes=['sb:wo'])
                bcast(gt2b, gt2, dg, 'sb:gt2b', ['sb:modT'])
                bcast(g2b, lambda kt: g2T[:, kt:kt + 1], dg, 'sb:g2b', ['sb:g2T'])
                bcast(sh2b, sh2, dg, 'sb:sh2b', ['sb:modT'])
                S.op('pool', lambda e: e.memset(selacc[:, :], 0.0), writes=['sb:selacc'])
                Xgv = Xg.rearrange("(n p) d -> p n d", p=128)
                def d_big(i):
                        xb = xt[i % 2]
                        xn_ = 'sb:xtd%d' % (i % 2)
                        h2f, h2T, lg, rt = h2f2[i % 2], h2T2[i % 2], lg2[i % 2], rt2[i % 2]
                        pz = str(i % 2)
                        smd = lambda kk, n=1, i=i: sm[:, kk + 26 * (i % 2):kk + 26 * (i % 2) + n]
                        hb = h2b[i % 2]
                        hbn = 'sb:h2b%d' % (i % 2)
                        S.dma('sp', lambda e, xb=xb, i=i: e.dma_start(out=xb[:, :], in_=x_src[i * 128:(i + 1) * 128, :]), reads=[xres], writes=[xn_])
                        mt = mts[i % 2]
                        mtn = 'sb:mt%d' % (i % 2)
                        S.dma('sp', lambda e, mt=mt, i=i: e.dma_start(out=mt[:, :, :], in_=mixTd[:, :, i * 128:(i + 1) * 128].rearrange("k p s -> p k s")),
                              reads=['dr:mixTd'], writes=[mtn])
                        for n4 in range(4):
                            S.group('pe', [lambda e, k=k, mt=mt, n4=n4: e.matmul(ps[n4][:, :], lhsT=mt[:, k, :], rhs=wo[:, k, n4 * 512:(n4 + 1) * 512],
                                                                               start=(k == 0), stop=(k == KT - 1)) for k in range(KT)],
                                    reads=[mtn, 'sb:wo'], writes=[P[n4]])

                def d_post(i):
                        xb = xt[i % 2]
                        xn_ = 'sb:xtd%d' % (i % 2)
                        h2f, h2T, lg, rt = h2f2[i % 2], h2T2[i % 2], lg2[i % 2], rt2[i % 2]
                        pz = str(i % 2)
                        smd = lambda kk, n=1, i=i: sm[:, kk + 26 * (i % 2):kk + 26 * (i % 2) + n]
                        hb = h2b[i % 2]
                        hbn = 'sb:h2b%d' % (i % 2)
                        for n4 in range(4):
                            S.op('dve', lambda e, n4=n4, xb=xb: e.tensor_tensor(out=xb[:, n4 * 512:(n4 + 1) * 512], in0=ps[n4][:, :], in1=xb[:, n4 * 512:(n4 + 1) * 512], op=ALU.add),
                                 reads=[P[n4], xn_], writes=[xn_])
                        S.dma('sp', lambda e, xb=xb, i=i: e.dma_start(out=xd[i * 128:(i + 1) * 128, :], in_=xb[:, :]), reads=[xn_], writes=['dr:xd_w'], acc=True)
                        S.op('act', lambda e, xb=xb: e.activation(out=junk[:, :], in_=xb[:, :], func=AF.Square, accum_out=smd(8)),
                             reads=[xn_], writes=['sb:junkd', 'sb:n2' + pz + '_ss'])
                        rstd_from_ss(smd(8), smd(9), D_, 'n2' + pz)
                        S.op('dve', lambda e, xb=xb: e.scalar_tensor_tensor(out=tmpx[:, :], in0=xb[:, :], scalar=smd(9), in1=g2b[:, :], op0=ALU.mult, op1=ALU.mult),
                             reads=[xn_, 'sb:n2' + pz + '_rs', 'sb:g2b'], writes=['sb:tmpx0', 'sb:tmpx1', 'sb:tmpx2', 'sb:tmpx3'])
                        S.op('dve', lambda e: e.tensor_tensor(out=h2f[:, :], in0=tmpx[:, :], in1=sh2b[:, :], op=ALU.add),
                             reads=['sb:tmpx0', 'sb:tmpx1', 'sb:tmpx2', 'sb:tmpx3', 'sb:sh2b'], writes=['sb:h2f' + pz])
                        hb = h2b[i % 2]
                        hbn = 'sb:h2b%d' % (i % 2)
                        S.op('act', lambda e, hb=hb: e.activation(out=hb[:, :], in_=h2f[:, :], func=AF.Copy), reads=['sb:h2f' + pz], writes=[hbn])

                def d_rA(i):
                        xb = xt[i % 2]
                        xn_ = 'sb:xtd%d' % (i % 2)
                        h2f, h2T, lg, rt = h2f2[i % 2], h2T2[i % 2], lg2[i % 2], rt2[i % 2]
                        pz = str(i % 2)
                        smd = lambda kk, n=1, i=i: sm[:, kk + 26 * (i % 2):kk + 26 * (i % 2) + n]
                        hb = h2b[i % 2]
                        hbn = 'sb:h2b%d' % (i % 2)
                        for g4 in range(4):
                            bank = 4 + g4 % 2
                            S.group('pe', [lambda e, kt=kt, bank=bank: e.transpose(ps[bank][:, (kt % 4) * 128:(kt % 4 + 1) * 128], h2f[:, kt * 128:(kt + 1) * 128], ident_f)
                                           for kt in range(g4 * 4, g4 * 4 + 4)], reads=['sb:h2f' + pz, 'sb:cst_f'], writes=[P[bank]])
                            S.op('act', lambda e, g4=g4, bank=bank: e.activation(out=h2T[:, g4 * 4:(g4 + 1) * 4, :], in_=ps[bank][:, :].rearrange("p (a t) -> p a t", a=4), func=AF.Copy),
                                 reads=[P[bank]], writes=['sb:h2T' + pz])
                        mm = [lambda e, k=k: e.matmul(ps[6][:, 0:36], lhsT=h2T[:, k, :], rhs=wr[:, l, k, :], start=(k == 0), stop=False) for k in range(KT)]
                        mm.append(lambda e: e.matmul(ps[6][:, 0:36], lhsT=ones_f[0:1, :], rhs=br[0:1, l, :], start=False, stop=True))
                        S.group('pe', mm, reads=['sb:h2T' + pz, 'sb:wr', 'sb:br', 'sb:cst_f'], writes=[P[6]])
                def d_rB(i):
                        xb = xt[i % 2]
                        xn_ = 'sb:xtd%d' % (i % 2)
                        h2f, h2T, lg, rt = h2f2[i % 2], h2T2[i % 2], lg2[i % 2], rt2[i % 2]
                        pz = str(i % 2)
                        smd = lambda kk, n=1, i=i: sm[:, kk + 26 * (i % 2):kk + 26 * (i % 2) + n]
                        hb = h2b[i % 2]
                        hbn = 'sb:h2b%d' % (i % 2)
                        R_ = ['sb:rt' + pz]
                        S.op('dve', lambda e: e.tensor_copy(out=lg[:, :], in_=ps[6][:, 0:36]), reads=[P[6]], writes=['sb:lg' + pz])
                        S.op('dve', lambda e: e.tensor_reduce(out=smd(10), in_=lg[:, 0:4], axis=AX.X, op=ALU.max), reads=['sb:lg' + pz], writes=R_)
                        S.op('dve', lambda e: e.tensor_scalar(out=rt[:, 0, 0:4], in0=lg[:, 0:4], scalar1=smd(10), scalar2=None, op0=ALU.is_ge), reads=['sb:lg' + pz] + R_, writes=R_)
                        S.op('dve', lambda e: e.tensor_scalar(out=smd(11), in0=smd(10), scalar1=-1.0, scalar2=None, op0=ALU.mult), reads=R_, writes=R_)
                        S.op('act', lambda e: e.activation(out=rt[:, 0, 8:12], in_=lg[:, 0:4], func=AF.Exp, bias=smd(11), accum_out=smd(12)), reads=['sb:lg' + pz] + R_, writes=R_)
                        S.op('dve', lambda e: e.tensor_scalar(out=rt[:, 0, 4:8], in0=rt[:, 0, 0:4], scalar1=-NEG, scalar2=NEG, op0=ALU.mult, op1=ALU.add), reads=R_, writes=R_)
                        S.op('dve', lambda e: e.tensor_tensor(out=rt[:, 1, :].rearrange("p (g j) -> p g j", g=4), in0=lg[:, 4:36].rearrange("p (g j) -> p g j", g=4),
                                                              in1=rt[:, 0, 4:8].unsqueeze(2).to_broadcast([128, 4, 8]), op=ALU.add), reads=['sb:lg' + pz] + R_, writes=R_)
                        S.op('dve', lambda e: e.max(out=rt[:, 0, 16:24], in_=rt[:, 1, :]), reads=R_, writes=R_)
                        v0 = rt[:, 0, 16:17]
                        v1 = rt[:, 0, 17:18]
                        S.op('dve', lambda e: e.tensor_scalar(out=rt[:, 2, :], in0=rt[:, 1, :], scalar1=v0, scalar2=None, op0=ALU.is_equal), reads=R_, writes=R_)
                        S.op('dve', lambda e: e.tensor_scalar(out=rt[:, 3, :], in0=rt[:, 1, :], scalar1=v1, scalar2=None, op0=ALU.is_equal), reads=R_, writes=R_)
                        S.op('dve', lambda e: e.tensor_tensor(out=smd(13), in0=v1, in1=v0, op=ALU.subtract), reads=R_, writes=R_)
                        S.op('act', lambda e: e.activation(out=smd(14), in_=smd(13), func=AF.Exp), reads=R_, writes=R_)
                        S.op('dve', lambda e: e.scalar_tensor_tensor(out=smd(15), in0=smd(14), scalar=1.0, in1=smd(12), op0=ALU.add, op1=ALU.mult), reads=R_, writes=R_)
                        S.op('dve', lambda e: e.reciprocal(out=wts[:, i, 0:1], in_=smd(15)), reads=R_, writes=['sb:wts'])
                        S.op('dve', lambda e: e.tensor_tensor(out=wts[:, i, 1:2], in0=wts[:, i, 0:1], in1=smd(14), op=ALU.mult), reads=['sb:wts'] + R_, writes=['sb:wts'])
                        S.op('dve', lambda e: e.tensor_tensor(out=rt[:, 4, :], in0=rt[:, 2, :], in1=rt[:, 3, :], op=ALU.add), reads=R_, writes=R_)
                        S.group('pe', [lambda e: e.matmul(ps[7][:, 0:32], lhsT=triS_f, rhs=rt[:, 4, :], start=True, stop=False),
                                       lambda e: e.matmul(ps[7][:, 0:32], lhsT=ones_f, rhs=selacc[:, :], start=False, stop=True)],
                                reads=R_ + ['sb:selacc', 'sb:cst_f'], writes=[P[7]])
                        S.op('dve', lambda e: e.tensor_tensor(out=selacc[:, :], in0=selacc[:, :], in1=rt[:, 4, :], op=ALU.add), reads=R_ + ['sb:selacc'], writes=['sb:selacc'])
                        S.op('dve', lambda e: e.tensor_scalar(out=rt[:, 5, :], in0=ps[7][:, 0:32], scalar1=float(CAP), scalar2=1.0e6, op0=ALU.is_ge, op1=ALU.mult), reads=[P[7]], writes=R_)
                        S.op('dve', lambda e: e.tensor_tensor(out=rt[:, 6, :], in0=ps[7][:, 0:32], in1=ebase[:, :], op=ALU.add), reads=[P[7], 'sb:ebase'], writes=R_)
                        S.op('dve', lambda e: e.tensor_tensor(out=rt[:, 6, :], in0=rt[:, 6, :], in1=rt[:, 5, :], op=ALU.add), reads=R_, writes=R_)
                        for a, idx in ((2, idx_a), (3, idx_b)):
                            S.op('dve', lambda e, a=a: e.tensor_tensor(out=rt[:, 7, :], in0=rt[:, a, :], in1=rt[:, 6, :], op=ALU.mult), reads=R_, writes=R_)
                            S.op('dve', lambda e: e.tensor_reduce(out=smd(16), in_=rt[:, 7, :], axis=AX.X, op=ALU.add), reads=R_, writes=R_)
                            S.op('dve', lambda e, idx=idx, i=i: e.tensor_copy(out=idx[:, i:i + 1], in_=smd(16)), reads=R_, writes=['sb:idx'])
                        for idx in (idx_a, idx_b):
                            S.dma('pool', lambda e, idx=idx, hb=hb, i=i: e.indirect_dma_start(
                                out=Xg[:, :], out_offset=bass.IndirectOffsetOnAxis(ap=idx[:, i:i + 1], axis=0),
                                in_=hb[:, :], in_offset=None, bounds_check=bc_reg, oob_is_err=False),
                                reads=[hbn, 'sb:idx'], writes=['dr:Xg'], slot='st_h2b%d' % (i % 2), acc=True)
                d_big(0)
                d_post(0)
                d_big(1)
                d_rA(0)
                for i in range(NT):
                    if i + 1 < NT:
                        d_post(i + 1)
                    if i + 2 < NT:
                        d_big(i + 2)
                    d_rB(i)
                    if i + 1 < NT:
                        d_rA(i + 1)
                S.barrier()
            if stop_after == 'D':
                if debug:
                    d = dbg_out("dbg_idx", [128, 2, NT], I32)
                    S.dma('sp', lambda e: e.dma_start(out=d[:, 0, :], in_=idx_a[:, :]), reads=['sb:idx'], writes=['dr:dbg_idx'])
                    S.dma('sp', lambda e: e.dma_start(out=d[:, 1, :], in_=idx_b[:, :]), reads=['sb:idx'], writes=['dr:dbg_idx2'])
                    d2 = dbg_out("dbg_wts", [128, NT, 2])
                    S.dma('sp', lambda e: e.dma_start(out=d2[:, :, :], in_=wts[:, :, :]), reads=['sb:wts'], writes=['dr:dbg_wts'])
                    d3 = dbg_out("dbg_x1", [S_, D_])
                    S.dma('sp', lambda e: e.dma_start(out=d3[:, :], in_=xd[:, :]), reads=['dr:xd_w'], writes=['dr:dbg_x1'])
                S.finish()
                return nc, dbg

            NST = CAP // 128
            NRING = 6
            with contextlib.ExitStack() as es:
                ring = [es.enter_context(SBT("wr%d" % i, [128, 16 * 512], BF16)) for i in range(NRING)]
                xe = [es.enter_context(SBT("xe%d" % i, [128, NST, D_], BF16)) for i in range(2)]
                xeT = es.enter_context(SBT("xeT", [128, KT, CAP], BF16))
                sgu = es.enter_context(SBT("sgu", [128, CAP], F32))
                hTe = [es.enter_context(SBT("hTe%d" % i, [128, 8, CAP], BF16)) for i in range(2)]
                yb_ = [es.enter_context(SBT("yb%d" % i, [128, 1024], F32)) for i in range(2)]
                pieces = []
                for e_ in range(NEXP):
                    for ms in range(2):
                        pieces.append((e_, 'g', ms))
                        pieces.append((e_, 'u', ms))
                    for ns in range(2):
                        pieces.append((e_, 'd', ns))

                def load_piece(pi):
                    e_, kind, hf = pieces[pi]
                    buf = ring[pi % NRING]
                    rn = 'sb:wr%d' % (pi % NRING)
                    if kind in 'gu':
                        src = (w_eg if kind == 'g' else w_eu)[l, e_].rearrange("(k p) n -> p k n", p=128)[:, :, hf * 512:(hf + 1) * 512]
                        bv = buf[:, :].rearrange("p (k n) -> p k n", k=16)
                        for q in range(2):
                            S.dma('pool', lambda e, bv=bv, src=src, q=q: e.dma_start(out=bv[:, q * 8:(q + 1) * 8, :], in_=src[:, q * 8:(q + 1) * 8, :]),
                                  reads=['dr:w_e'], writes=[rn], acc=(q > 0))
                    else:
                        src = w_ed[l, e_].rearrange("(k p) n -> p k n", p=128)[:, :, hf * 1024:(hf + 1) * 1024]
                        bv = buf[:, :].rearrange("p (k n) -> p k n", k=8)
                        for q in range(2):
                            S.dma('pool', lambda e, bv=bv, src=src, q=q: e.dma_start(out=bv[:, q * 4:(q + 1) * 4, :], in_=src[:, q * 4:(q + 1) * 4, :]),
                                  reads=['dr:w_e'], writes=[rn], acc=(q > 0))
                LOOK = NRING - 1
                for pi in range(LOOK):
                    load_piece(pi)

                def load_xe(e_):
                    S.dma('sp', lambda e, e_=e_: e.dma_start(out=xe[e_ % 2][:, :, :], in_=Xg[e_ * CAP:(e_ + 1) * CAP, :].rearrange("(s p) d -> p s d", p=128)),
                          reads=['dr:Xg'], writes=['sb:xe%d' % (e_ % 2)])
                load_xe(0)
                pi = 0
                ycnt = 0
                for e_ in range(NEXP):
                    if e_ + 1 < NEXP:
                        load_xe(e_ + 1)
                    xb = xe[e_ % 2]
                    xbn = 'sb:xe%d' % (e_ % 2)
                    xT = xeT
                    xTn = 'sb:xeT'
                    hE = hTe[e_ % 2]
                    hEn = 'sb:hTe%d' % (e_ % 2)
                    KPB = 1024 // CAP
                    for g in range(KT // KPB):
                        bank = g % 2
                        tp = ps[bank][:, :].bitcast(BF16)
                        S.group('pe', [lambda e, kt=kt, st=st, tp=tp: e.transpose(tp[:, (kt % KPB) * CAP + st * 128:(kt % KPB) * CAP + (st + 1) * 128],
                                                                                 xb[:, st, kt * 128:(kt + 1) * 128], ident_b)
                                       for kt in range(g * KPB, (g + 1) * KPB) for st in range(NST)], reads=[xbn, 'sb:cst_b'], writes=[P[bank]])
                        if g % 2 == 0:
                            S.op('dve', lambda e, g=g, tp=tp: e.tensor_copy(out=xT[:, g * KPB:(g + 1) * KPB, :], in_=tp.rearrange("p (k s) -> p k s", k=KPB)),
                                 reads=[P[bank]], writes=[xTn])
                        else:
                            S.op('act', lambda e, g=g, tp=tp: e.activation(out=xT[:, g * KPB:(g + 1) * KPB, :], in_=tp.rearrange("p (k s) -> p k s", k=KPB), func=AF.Copy),
                                 reads=[P[bank]], writes=[xTn])
                    gu = 0
                    for ms in range(2):
                        bg = ring[pi % NRING][:, :].rearrange("p (k n) -> p k n", k=16)
                        bgn = 'sb:wr%d' % (pi % NRING)
                        bu = ring[(pi + 1) % NRING][:, :].rearrange("p (k n) -> p k n", k=16)
                        bun = 'sb:wr%d' % ((pi + 1) % NRING)
                        for m in range(4):
                            bkg = 2 + 2 * (gu % 2)
                            bku = bkg + 1
                            gu += 1
                            S.group('pe', [lambda e, k=k, m=m, bkg=bkg, bg=bg: e.matmul(ps[bkg][:, 0:CAP], lhsT=bg[:, k, m * 128:(m + 1) * 128], rhs=xT[:, k, :],
                                                                                       start=(k == 0), stop=(k == KT - 1)) for k in range(KT)],
                                    reads=[bgn, xTn], writes=[P[bkg]])
                            S.group('pe', [lambda e, k=k, m=m, bku=bku, bu=bu: e.matmul(ps[bku][:, 0:CAP], lhsT=bu[:, k, m * 128:(m + 1) * 128], rhs=xT[:, k, :],
                                                                                       start=(k == 0), stop=(k == KT - 1)) for k in range(KT)],
                                    reads=[bun, xTn], writes=[P[bku]])
                            S.op('act', lambda e, bkg=bkg: e.activation(out=sgu[:, :], in_=ps[bkg][:, 0:CAP], func=AF.Silu), reads=[P[bkg]], writes=['sb:sgu'])
                            S.op('dve', lambda e, bku=bku, ms=ms, m=m: e.tensor_tensor(out=hE[:, ms * 4 + m, :], in0=ps[bku][:, 0:CAP], in1=sgu[:, :], op=ALU.mult),
                                 reads=[P[bku], 'sb:sgu'], writes=[hEn])
                        pi += 2
                        for q in range(2):
                            if pi - 2 + q + LOOK < len(pieces):
                                load_piece(pi - 2 + q + LOOK)
                    for ns in range(2):
                        bd = ring[pi % NRING][:, :].rearrange("p (k n) -> p k n", k=8)
                        bdn = 'sb:wr%d' % (pi % NRING)
                        for st in range(NST):
                            yb = yb_[ycnt % 2]
                            ybn = 'sb:yb%d' % (ycnt % 2)
                            ycnt += 1
                            for nn in range(2):
                                bank = 6 + nn
                                S.group('pe', [lambda e, k=k, st=st, nn=nn, bank=bank, bd=bd: e.matmul(ps[bank][:, :], lhsT=hE[:, k, st * 128:(st + 1) * 128], rhs=bd[:, k, nn * 512:(nn + 1) * 512],
                                                                                                 start=(k == 0), stop=(k == 7)) for k in range(8)],
                                        reads=[bdn, hEn], writes=[P[bank]])
                                if nn == 0:
                                    S.op('act', lambda e, bank=bank, yb=yb: e.activation(out=yb[:, 0:512], in_=ps[bank][:, :], func=AF.Copy), reads=[P[bank]], writes=[ybn])
                                else:
                                    S.op('dve', lambda e, bank=bank, yb=yb: e.tensor_copy(out=yb[:, 512:1024], in_=ps[bank][:, :]), reads=[P[bank]], writes=[ybn])
                            S.dma('sp', lambda e, st=st, e_=e_, ns=ns, yb=yb: e.dma_start(
                                out=Yg[e_ * CAP + st * 128:e_ * CAP + (st + 1) * 128, ns * 1024:(ns + 1) * 1024], in_=yb[:, :]),
                                reads=[ybn], writes=['dr:Yg'], acc=True)
                        pi += 1
                        if pi - 1 + LOOK < len(pieces):
                            load_piece(pi - 1 + LOOK)
                S.barrier()

            with contextlib.ExitStack() as es:
                ya2 = [es.enter_context(SBT("ya%d" % z, [128, D_], F32)) for z in range(2)]
                yb22 = [es.enter_context(SBT("yb2%d" % z, [128, D_], F32)) for z in range(2)]
                xt = [es.enter_context(SBT("xtf%d" % i, [128, D_], F32)) for i in range(2)]
                junk = es.enter_context(SBT("junkf", [128, D_], BF16))
                if last:
                    lnfb = es.enter_context(SBT("lnfb", [128, D_], F32))
                    bcast(lnfb, lambda kt: lnT[:, 2 * DEPTH, kt:kt + 1], dg, 'sb:lnfb', ['sb:lnT'])
                for i in range(NT):
                    ya, yb2, pz = ya2[i % 2], yb22[i % 2], str(i % 2)
                    xb = xt[i % 2]
                    xn_ = 'sb:xtf%d' % (i % 2)
                    S.dma('sp', lambda e, xb=xb, i=i: e.dma_start(out=xb[:, :], in_=xd[i * 128:(i + 1) * 128, :]), reads=['dr:xd_w'], writes=[xn_])
                    if i < 2:
                        S.op('dve', lambda e: e.memset(ya[:, :], 0.0), writes=['sb:ya' + pz])
                        S.op('dve', lambda e: e.memset(yb2[:, :], 0.0), writes=['sb:yb2' + pz])
                    for idx, yt, yn in ((idx_a, ya, 'sb:ya' + pz), (idx_b, yb2, 'sb:yb2' + pz)):
                        S.dma('pool', lambda e, idx=idx, yt=yt, i=i: e.indirect_dma_start(
                            out=yt[:, :], out_offset=None, in_=Yg[:, :], in_offset=bass.IndirectOffsetOnAxis(ap=idx[:, i:i + 1], axis=0),
                            bounds_check=bc_reg, oob_is_err=False), reads=['dr:Yg', 'sb:idx'], writes=[yn])
                    S.op('act', lambda e, i=i: e.activation(out=yb2[:, :], in_=yb2[:, :], func=AF.Copy, scale=wts[:, i, 1:2]),
                         reads=['sb:yb2' + pz, 'sb:wts'], writes=['sb:yb2' + pz])
                    S.op('dve', lambda e, i=i: e.scalar_tensor_tensor(out=ya[:, :], in0=ya[:, :], scalar=wts[:, i, 0:1], in1=yb2[:, :], op0=ALU.mult, op1=ALU.add),
                         reads=['sb:ya' + pz, 'sb:yb2' + pz, 'sb:wts'], writes=['sb:ya' + pz])
                    S.op('dve', lambda e: e.tensor_tensor(out=ya[:, :], in0=ya[:, :], in1=gt2b[:, :], op=ALU.mult), reads=['sb:ya' + pz, 'sb:gt2b'], writes=['sb:ya' + pz])
                    S.op('dve', lambda e, xb=xb: e.tensor_tensor(out=xb[:, :], in0=xb[:, :], in1=ya[:, :], op=ALU.add), reads=['sb:ya' + pz, xn_], writes=[xn_])
                    if not last:
                        S.dma('sp', lambda e, xb=xb, i=i: e.dma_start(out=xd[i * 128:(i + 1) * 128, :], in_=xb[:, :]), reads=[xn_], writes=['dr:xd'], acc=True)
                    else:
                        S.op('act', lambda e, xb=xb: e.activation(out=junk[:, :], in_=xb[:, :], func=AF.Square, accum_out=smc(20)),
                             reads=[xn_], writes=['sb:junkf', 'sb:nf_ss'])
                        rstd_from_ss(smc(20), smc(21), D_, 'nf')
                        S.op('dve', lambda e, xb=xb: e.scalar_tensor_tensor(out=xb[:, :], in0=xb[:, :], scalar=smc(21), in1=lnfb[:, :], op0=ALU.mult, op1=ALU.mult),
                             reads=[xn_, 'sb:nf_rs', 'sb:lnfb'], writes=[xn_])
                        S.dma('sp', lambda e, xb=xb, i=i: e.dma_start(out=y_out[i * 128:(i + 1) * 128, :], in_=xb[:, :]), reads=[xn_], writes=['dr:y'], acc=True)
                S.barrier()
    S.finish()
    return nc, dbg


def _consts():
    p = np.arange(128)[:, None]
    f = np.arange(128)[None, :]
    c = np.zeros((128, 6, 128), np.float32)
    c[:, 0] = (p == f)
    c[:, 1] = (p <= f)
    c[:, 2] = (p > f)
    c[:, 3] = (p < f)
    c[:, 4] = 1.0
    half = 16
    inv = np.power(np.float32(500000.0), -np.arange(half, dtype=np.float32) * np.float32(2.0 / 32)).astype(np.float32)
    ang = (np.arange(S_, dtype=np.float32)[:, None] * inv[None, :]).astype(np.float32)
    cos = np.cos(ang).astype(np.float32)
    sin = np.sin(ang).astype(np.float32)
    cs = np.concatenate([cos, cos], axis=1)
    sn = np.concatenate([-sin, sin], axis=1)
    rope = np.zeros((S_, 2, 2, 32), np.float32)
    rope[:, 0, 0] = cs * np.float32(ATTN_SCALE)
    rope[:, 0, 1] = cs
    rope[:, 1, 0] = sn * np.float32(ATTN_SCALE)
    rope[:, 1, 1] = sn
    rope = np.ascontiguousarray(rope.reshape(NT, 128, 2, 2, 32).transpose(1, 0, 2, 3, 4))
    negm = np.zeros((128, 8, 8), np.float32)
    for blk in range(8):
        negm[:, blk, blk:] = NEG
    ebase = np.broadcast_to((np.arange(NEXP, dtype=np.float32) * CAP)[None, :], (128, NEXP)).copy()
    return c, rope, negm, ebase


def _fm(v):
    v = np.asarray(v, np.float32)
    lead = v.shape[:-1]
    return np.ascontiguousarray(np.moveaxis(v.reshape(*lead, -1, 128), -1, 0))


_NC_CACHE = {}


def make_inputs(x, c, ln1, ln2, w_ada, b_ada, w_in, w_gk, b_gk, g_onorm, w_out, w_r1, b_r1, w_r2, b_r2,
                w_e_gate, w_e_up, w_e_down, ln_f):
    f = lambda a: np.ascontiguousarray(np.asarray(a, dtype=np.float32))
    x = f(x)
    consts, rope, negm, ebase = _consts()
    lnT = np.stack([_fm(ln1[0]), _fm(ln2[0]), _fm(ln1[1]), _fm(ln2[1]), _fm(ln_f)], axis=1)
    badaT = _fm(np.asarray(b_ada, np.float32))
    wgk_aug = np.ascontiguousarray(np.concatenate([np.asarray(w_gk, np.float32), np.asarray(b_gk, np.float32)[:, None, :]], axis=1).transpose(1, 0, 2))
    gon_b = np.ascontiguousarray(np.broadcast_to(np.asarray(g_onorm, np.float32)[None], (128, DEPTH, 256)))
    wrc = np.concatenate([np.asarray(w_r1, np.float32), np.asarray(w_r2, np.float32)], axis=2)
    wr = np.ascontiguousarray(wrc.reshape(DEPTH, KT, 128, 36).transpose(2, 0, 1, 3))
    br = np.ascontiguousarray(np.concatenate([np.asarray(b_r1, np.float32), np.asarray(b_r2, np.float32)], axis=1)[None])
    shared = dict(lnT=np.ascontiguousarray(lnT), badaT=badaT, w_ada=f(w_ada), w_in=f(w_in), wgk_aug=wgk_aug, gon_b=gon_b,
                  w_out=f(w_out), wr=wr, br=br, w_e_gate=f(w_e_gate), w_e_up=f(w_e_up), w_e_down=f(w_e_down),
                  rope=rope, consts=consts, negmask=negm, ebase=ebase)
    in_maps = []
    for b in range(NCORES):
        m = dict(shared)
        m["x"] = x[b]
        m["cT"] = _fm(np.asarray(c, np.float32)[b])
        in_maps.append(m)
    return in_maps


def kernel(**inputs):
    in_maps = make_inputs(**inputs)
    if 'nc' not in _NC_CACHE:
        _NC_CACHE['nc'] = build_nc()[0]
    res = run_bass_kernel_spmd(_NC_CACHE['nc'], in_maps, core_ids=list(range(NCORES)))
    return np.stack([np.asarray(r["y"], dtype=np.float32) for r in res.results], axis=0)
```
